# Optimizing a Trainium2 kernel written in Bass

```python
import math
import jax, jax.numpy as jnp
from jax import lax
import numpy as np

D_MODEL = 1024
BATCH = 2
SEQ = 8192
DEPTH = 2

N_A_LAYERS = DEPTH // 2
N_B_LAYERS = DEPTH - N_A_LAYERS

MLSTM_HEADS = 4
MLSTM_INNER = 2 * D_MODEL
MLSTM_HEAD_DIM = MLSTM_INNER // MLSTM_HEADS
CONV_WIDTH = 4
MLSTM_CHUNK = 128

ATTN_HEADS = 16
ATTN_HEAD_DIM = D_MODEL // ATTN_HEADS
MOBA_BLOCK = 256
MOBA_TOPK = 3
MOBA_QCHUNK = 32

N_EXPERTS = 16
N_GROUPS = 4
EXPERTS_PER_GROUP = N_EXPERTS // N_GROUPS
MOE_TOPK = 2
D_FF_EXPERT = 512
MOE_ROW_BLOCK = 256

DEEPNORM_ALPHA = (2.0 * DEPTH) ** 0.25
DEEPNORM_BETA = (8.0 * DEPTH) ** -0.25
LN_EPS = 1e-5
N_MOD_PER_LAYER = 6

kernel_name = 'hybrid_mlstm_moba_groupmoe'


def layer_norm(x, g, b):
    xf = x.astype(jnp.float32)
    mu = xf.mean(-1, keepdims=True)
    var = jnp.square(xf - mu).mean(-1, keepdims=True)
    return ((xf - mu) * lax.rsqrt(var + LN_EPS) * g + b).astype(x.dtype)


def modulate(x, shift, scale):
    return x * (1 + scale[:, None, :]) + shift[:, None, :]


def causal_conv(x, w, b):
    K, C = w.shape
    y = lax.conv_general_dilated(x, w[:, None, :], window_strides=(1,), padding=[(K - 1, 0)],
                                 dimension_numbers=('NWC', 'WIO', 'NWC'), feature_group_count=C)
    return y + b


def mlstm_chunkwise(q, k, v, log_i, log_f):
    B, H, S, Dh = q.shape
    L = MLSTM_CHUNK
    nc = S // L

    def to_chunks(t):
        return jnp.moveaxis(t.reshape(t.shape[:2] + (nc, L) + t.shape[3:]), 2, 0)

    causal = jnp.tril(jnp.ones((L, L), bool))

    def step(carry, inp):
        C, n, m = carry
        qc, kc, vc, ic, fc = inp
        b = jnp.cumsum(fc, axis=-1)
        d = jnp.where(causal, b[..., :, None] - b[..., None, :] + ic[..., None, :], -jnp.inf)
        a = b + m[..., None]
        m_t = jnp.maximum(a, d.max(-1))
        w_intra = jnp.exp(d - m_t[..., None])
        w_inter = jnp.exp(a - m_t)
        s = jnp.einsum('bhtd,bhsd->bhts', qc, kc) * w_intra
        num = jnp.einsum('bhts,bhsd->bhtd', s, vc) + w_inter[..., None] * jnp.einsum('bhvd,bhtd->bhtv', C, qc)
        den = s.sum(-1) + w_inter * jnp.einsum('bhd,bhtd->bht', n, qc)
        h = num / jnp.maximum(jnp.abs(den), jnp.exp(-m_t))[..., None]
        b_last = b[..., -1]
        w_s = b_last[..., None] - b + ic
        m_new = jnp.maximum(b_last + m, w_s.max(-1))
        decay = jnp.exp(b_last + m - m_new)
        w_s = jnp.exp(w_s - m_new[..., None])
        C_new = decay[..., None, None] * C + jnp.einsum('bhs,bhsv,bhsd->bhvd', w_s, vc, kc)
        n_new = decay[..., None] * n + jnp.einsum('bhs,bhsd->bhd', w_s, kc)
        return (C_new, n_new, m_new), h

    init = (jnp.zeros((B, H, Dh, Dh), jnp.float32), jnp.zeros((B, H, Dh), jnp.float32),
            jnp.zeros((B, H), jnp.float32))
    _, hs = lax.scan(step, init, (to_chunks(q), to_chunks(k), to_chunks(v), to_chunks(log_i), to_chunks(log_f)))
    return jnp.moveaxis(hs, 0, 2).reshape(B, H, S, Dh)


def mlstm_layer(h, w_in, conv_w, conv_b, wq, wk, wv, w_if, b_if, gn_g, skip, w_out):
    B, S, _ = h.shape
    NH, DH = MLSTM_HEADS, MLSTM_HEAD_DIM
    f32 = jnp.float32
    xm, z = jnp.split(h @ w_in, 2, axis=-1)
    xc = jax.nn.silu(causal_conv(xm, conv_w, conv_b))
    xc_h = xc.reshape(B, S, NH, DH)
    xm_h = xm.reshape(B, S, NH, DH)
    q = jnp.einsum('bshd,hde->bhse', xc_h, wq).astype(f32)
    k = (jnp.einsum('bshd,hde->bhse', xc_h, wk) * (DH ** -0.5)).astype(f32)
    v = jnp.einsum('bshd,hde->bhse', xm_h, wv).astype(f32)
    gates = jnp.transpose((xc @ w_if + b_if).astype(f32), (0, 2, 1))
    log_i = gates[:, :NH]
    log_f = jax.nn.log_sigmoid(gates[:, NH:])
    hh = mlstm_chunkwise(q, k, v, log_i, log_f)
    mu = hh.mean(-1, keepdims=True)
    var = jnp.square(hh - mu).mean(-1, keepdims=True)
    hh = (hh - mu) * lax.rsqrt(var + LN_EPS)
    hh = hh.transpose(0, 2, 1, 3).reshape(B, S, MLSTM_INNER).astype(h.dtype) * gn_g
    hh = (hh + skip * xc) * jax.nn.silu(z)
    return hh @ w_out


def shared_kv(x, shift, scale, w_kv):
    B, S, D = x.shape
    k, v = jnp.split(modulate(x, shift, scale) @ w_kv, 2, axis=-1)
    s_pad = -(-S // MOBA_BLOCK) * MOBA_BLOCK
    nblk = s_pad // MOBA_BLOCK

    def to_blocks(t):
        t = t.reshape(B, S, ATTN_HEADS, ATTN_HEAD_DIM).transpose(0, 2, 1, 3)
        t = jnp.pad(t, ((0, 0), (0, 0), (0, s_pad - S), (0, 0)))
        return t.reshape(B, ATTN_HEADS, nblk, MOBA_BLOCK, ATTN_HEAD_DIM)

    k_blocks, v_blocks = to_blocks(k), to_blocks(v)
    k_means = k_blocks.astype(jnp.float32).mean(3).astype(k.dtype)
    return k_blocks, v_blocks, k_means


def alibi_slopes(n_heads):
    return jnp.exp2(-8.0 * (jnp.arange(n_heads, dtype=jnp.float32) + 1.0) / n_heads)


def moba_attention(q, k_blocks, v_blocks, k_means, slopes):
    B, H, S, Dh = q.shape
    nblk = k_blocks.shape[2]
    topk = min(MOBA_TOPK, nblk)
    QC, BLK = MOBA_QCHUNK, MOBA_BLOCK
    nq = S // QC
    q_chunks = jnp.moveaxis(q.reshape(B, H, nq, QC, Dh), 2, 0)
    bi = jnp.arange(B)[:, None, None, None]
    hi = jnp.arange(H)[None, :, None, None]
    blk_ids = jnp.arange(nblk)
    offs = jnp.arange(BLK)
    f32 = jnp.float32

    def one_chunk(args):
        ci, qc = args
        t = ci * QC + jnp.arange(QC)
        own = (ci * QC) // BLK
        gate = jnp.einsum('bhtd,bhnd->bhtn', qc, k_means).astype(f32)
        gate = jnp.where(blk_ids < own, gate, -jnp.inf)
        _, sel = lax.top_k(gate, topk)
        valid = sel < own
        k_sel = k_blocks[bi, hi, sel]
        v_sel = v_blocks[bi, hi, sel].reshape(B, H, QC, topk * BLK, Dh)
        s_sel = jnp.einsum('bhtd,bhtjsd->bhtjs', qc, k_sel).astype(f32)
        pos_sel = sel[..., None] * BLK + offs
        dist_sel = (t[:, None, None] - pos_sel).astype(f32)
        s_sel = s_sel - slopes[:, None, None, None] * dist_sel
        s_sel = jnp.where(valid[..., None], s_sel, -jnp.inf).reshape(B, H, QC, topk * BLK)
        k_own = lax.dynamic_index_in_dim(k_blocks, own, axis=2, keepdims=False)
        v_own = lax.dynamic_index_in_dim(v_blocks, own, axis=2, keepdims=False)
        s_own = jnp.einsum('bhtd,bhsd->bhts', qc, k_own).astype(f32)
        pos_own = own * BLK + offs
        dist_own = (t[:, None] - pos_own[None, :]).astype(f32)
        s_own = jnp.where(dist_own >= 0, s_own - slopes[:, None, None] * dist_own, -jnp.inf)
        p = jax.nn.softmax(jnp.concatenate([s_sel, s_own], axis=-1), axis=-1).astype(v_blocks.dtype)
        return (jnp.einsum('bhts,bhtsd->bhtd', p[..., :topk * BLK], v_sel)
                + jnp.einsum('bhts,bhsd->bhtd', p[..., topk * BLK:], v_own))

    outs = lax.map(one_chunk, (jnp.arange(nq), q_chunks))
    return jnp.moveaxis(outs, 0, 2).reshape(B, H, S, Dh)


def moba_layer(h, wq, wo, k_blocks, v_blocks, k_means, slopes):
    B, S, D = h.shape
    q = (h @ wq).reshape(B, S, ATTN_HEADS, ATTN_HEAD_DIM).transpose(0, 2, 1, 3) * (ATTN_HEAD_DIM ** -0.5)
    o = moba_attention(q, k_blocks, v_blocks, k_means, slopes)
    return o.transpose(0, 2, 1, 3).reshape(B, S, D) @ wo


def route(x2, w_router, b_router):
    T = x2.shape[0]
    probs = jax.nn.softmax((x2 @ w_router).astype(jnp.float32), axis=-1)
    sel_scores = (probs + b_router).reshape(T, N_GROUPS, EXPERTS_PER_GROUP)
    grp_score = lax.top_k(sel_scores, MOE_TOPK)[0].sum(-1)
    g = jnp.argmax(grp_score, axis=-1)
    masked = jnp.where(jnp.arange(N_GROUPS)[None, :, None] == g[:, None, None], sel_scores, -jnp.inf)
    _, idx = lax.top_k(masked.reshape(T, N_EXPERTS), MOE_TOPK)
    w = jnp.take_along_axis(probs, idx, axis=-1)
    return idx, w / w.sum(-1, keepdims=True)


def moe_ffn(h, w_router, b_router, w_gate, w_up, w_down):
    B, S, D = h.shape
    T = B * S
    x2 = h.reshape(T, D)
    idx, wts = route(x2, w_router, b_router)
    n_assign = T * MOE_TOPK
    e_flat = idx.reshape(-1)
    tok_flat = jnp.repeat(jnp.arange(T, dtype=jnp.int32), MOE_TOPK)
    w_flat = wts.reshape(-1)
    order = jnp.argsort(e_flat)
    e_sorted = e_flat[order]
    counts = jnp.zeros((N_EXPERTS,), jnp.int32).at[e_flat].add(1)
    starts = jnp.cumsum(counts) - counts
    padded = (counts + MOE_ROW_BLOCK - 1) // MOE_ROW_BLOCK * MOE_ROW_BLOCK
    pends = jnp.cumsum(padded)
    pstarts = pends - padded
    dest = pstarts[e_sorted] + (jnp.arange(n_assign, dtype=jnp.int32) - starts[e_sorted])
    n_blocks = -(-n_assign // MOE_ROW_BLOCK) + N_EXPERTS
    cap = n_blocks * MOE_ROW_BLOCK
    buf_tok = jnp.zeros((cap,), jnp.int32).at[dest].set(tok_flat[order])
    buf_w = jnp.zeros((cap,), jnp.float32).at[dest].set(w_flat[order])
    block_start = jnp.arange(n_blocks, dtype=jnp.int32) * MOE_ROW_BLOCK
    block_expert = jnp.minimum(jnp.searchsorted(pends, block_start, side='right'), N_EXPERTS - 1)
    xs = x2[buf_tok].reshape(n_blocks, MOE_ROW_BLOCK, D)

    def expert_block(args):
        xb, e = args
        return (jax.nn.silu(xb @ w_gate[e]) * (xb @ w_up[e])) @ w_down[e]

    ys = lax.map(expert_block, (xs, block_expert)).reshape(cap, D)
    y = jnp.zeros((T, D), h.dtype).at[buf_tok].add(ys * buf_w[:, None].astype(ys.dtype))
    return y.reshape(B, S, D)


def setup_inputs(seed: int = 0) -> dict:
    key = jax.random.key(seed)
    ks = jax.random.split(key, 27)
    D, DI, NH, DH = D_MODEL, MLSTM_INNER, MLSTM_HEADS, MLSTM_HEAD_DIM
    nA, nB = N_A_LAYERS, N_B_LAYERS

    def nrm(k, shape, s):
        return jax.random.normal(k, shape, jnp.float32) * s

    n_mod = DEPTH * N_MOD_PER_LAYER * D + 2 * D
    x = nrm(ks[0], (BATCH, SEQ, D), 1.0)
    c = nrm(ks[1], (BATCH, D), 1.0)
    w_ada = nrm(ks[2], (D, n_mod), 0.1 * D ** -0.5)
    b_ada = nrm(ks[3], (n_mod,), 0.01)
    ln_g = 1.0 + nrm(ks[4], (DEPTH, 2, D), 0.02)
    ln_b = nrm(ks[5], (DEPTH, 2, D), 0.02)
    a_w_in = nrm(ks[6], (nA, D, 2 * DI), D ** -0.5)
    a_conv_w = nrm(ks[7], (nA, CONV_WIDTH, DI), CONV_WIDTH ** -0.5)
    a_conv_b = nrm(ks[8], (nA, DI), 0.01)
    a_wq = nrm(ks[9], (nA, NH, DH, DH), DH ** -0.5)
    a_wk = nrm(ks[10], (nA, NH, DH, DH), DH ** -0.5)
    a_wv = nrm(ks[11], (nA, NH, DH, DH), DEEPNORM_BETA * DH ** -0.5)
    a_w_if = nrm(ks[12], (nA, DI, 2 * NH), 0.1 * DI ** -0.5)
    a_b_if = jnp.concatenate([nrm(ks[13], (nA, NH), 0.1),
                              jnp.linspace(3.0, 6.0, NH) + nrm(ks[14], (nA, NH), 0.1)], axis=-1)
    a_gn_g = 1.0 + nrm(ks[15], (nA, DI), 0.02)
    a_skip = 1.0 + nrm(ks[16], (nA, DI), 0.02)
    a_w_out = nrm(ks[17], (nA, DI, D), DEEPNORM_BETA * DI ** -0.5)
    b_w_kv = jnp.concatenate([nrm(ks[18], (D, D), D ** -0.5),
                              nrm(ks[19], (D, D), DEEPNORM_BETA * D ** -0.5)], axis=-1)
    b_wq = nrm(ks[20], (nB, D, D), D ** -0.5)
    b_wo = nrm(ks[21], (nB, D, D), DEEPNORM_BETA * D ** -0.5)
    moe_w_router = nrm(ks[22], (D, N_EXPERTS), D ** -0.5)
    moe_b_router = nrm(ks[23], (N_EXPERTS,), 0.01)
    moe_w_gate = nrm(ks[24], (DEPTH, N_EXPERTS, D, D_FF_EXPERT), D ** -0.5)
    moe_w_up = nrm(ks[25], (DEPTH, N_EXPERTS, D, D_FF_EXPERT), D ** -0.5)
    moe_w_down = nrm(ks[26], (DEPTH, N_EXPERTS, D_FF_EXPERT, D), DEEPNORM_BETA * D_FF_EXPERT ** -0.5)
    return {'x': x, 'c': c, 'w_ada': w_ada, 'b_ada': b_ada, 'ln_g': ln_g, 'ln_b': ln_b,
            'a_w_in': a_w_in, 'a_conv_w': a_conv_w, 'a_conv_b': a_conv_b, 'a_wq': a_wq, 'a_wk': a_wk,
            'a_wv': a_wv, 'a_w_if': a_w_if, 'a_b_if': a_b_if, 'a_gn_g': a_gn_g, 'a_skip': a_skip,
            'a_w_out': a_w_out, 'b_w_kv': b_w_kv, 'b_wq': b_wq, 'b_wo': b_wo,
            'moe_w_router': moe_w_router, 'moe_b_router': moe_b_router, 'moe_w_gate': moe_w_gate,
            'moe_w_up': moe_w_up, 'moe_w_down': moe_w_down}


def reference(x, c, w_ada, b_ada, ln_g, ln_b, a_w_in, a_conv_w, a_conv_b, a_wq, a_wk, a_wv, a_w_if,
              a_b_if, a_gn_g, a_skip, a_w_out, b_w_kv, b_wq, b_wo, moe_w_router, moe_b_router,
              moe_w_gate, moe_w_up, moe_w_down):
    B, S, D = x.shape
    n_layer_mod = DEPTH * N_MOD_PER_LAYER * D
    cond = jax.nn.silu(c) @ w_ada + b_ada
    mods = cond[:, :n_layer_mod].reshape(B, DEPTH, 2, 3, D)
    kv_mod = cond[:, n_layer_mod:].reshape(B, 2, D)
    slopes = alibi_slopes(ATTN_HEADS)
    kv = None
    for layer in range(DEPTH):
        shift, scale, gate = mods[:, layer, 0, 0], mods[:, layer, 0, 1], 1.0 + mods[:, layer, 0, 2]
        h = modulate(x, shift, scale)
        if layer < N_A_LAYERS:
            i = layer
            y = mlstm_layer(h, a_w_in[i], a_conv_w[i], a_conv_b[i], a_wq[i], a_wk[i], a_wv[i], a_w_if[i],
                            a_b_if[i], a_gn_g[i], a_skip[i], a_w_out[i])
        else:
            if kv is None:
                kv = shared_kv(x, kv_mod[:, 0], kv_mod[:, 1], b_w_kv)
            j = layer - N_A_LAYERS
            y = moba_layer(h, b_wq[j], b_wo[j], kv[0], kv[1], kv[2], slopes)
        x = layer_norm(DEEPNORM_ALPHA * x + gate[:, None, :] * y, ln_g[layer, 0], ln_b[layer, 0])
        shift, scale, gate = mods[:, layer, 1, 0], mods[:, layer, 1, 1], 1.0 + mods[:, layer, 1, 2]
        h = modulate(x, shift, scale)
        y = moe_ffn(h, moe_w_router, moe_b_router, moe_w_gate[layer], moe_w_up[layer], moe_w_down[layer])
        x = layer_norm(DEEPNORM_ALPHA * x + gate[:, None, :] * y, ln_g[layer, 1], ln_b[layer, 1])
    return x
```

```python
import numpy as np
import ml_dtypes
import concourse.bass as bass
import concourse.mybir as mybir
from concourse.bass_utils import run_bass_kernel_spmd

F32 = mybir.dt.float32
BF16 = mybir.dt.bfloat16
AF = mybir.ActivationFunctionType
ALU = mybir.AluOpType
AX = mybir.AxisListType
NPBF = ml_dtypes.bfloat16

D_MODEL = 1024
SEQ = 8192
NCORES = 8
LN_EPS = 1e-5
ALPHA = 4.0 ** 0.25
BIG = 30000.0


class Buf:
    __slots__ = ("t", "name", "w", "r", "dma_sem", "dma_cnt", "psum")

    def __init__(self, t, name):
        self.t = t
        self.name = name
        self.psum = False
        self.w = None
        self.r = []
        self.dma_sem = None
        self.dma_cnt = 0

    def __getitem__(self, k):
        return self.t[k]


class Ring:
    def __init__(self, bufs):
        self.bufs = bufs
        self.i = 0

    def next(self):
        b = self.bufs[self.i % len(self.bufs)]
        self.i += 1
        return b


class FW:
    def __init__(self, nc):
        self.nc = nc
        self.stack = []
        self.sem_stack = []
        self.eng = {"pe": nc.tensor, "act": nc.scalar, "dve": nc.vector, "pool": nc.gpsimd, "sp": nc.sync}
        self.sem = {}
        self.cnt = {}
        for k in self.eng:
            self.sem[k] = self._enter_sem(nc.semaphore("s_" + k))
            self.cnt[k] = 0
        self.known = {e: {} for e in self.eng}
        self.nbuf = 0
        self.n_inst = 0
        self.live = []

    def _enter(self, cm):
        v = cm.__enter__()
        self.stack.append(cm)
        return v

    def _enter_sem(self, cm):
        v = cm.__enter__()
        self.sem_stack.append(cm)
        return v

    def close(self):
        while self.stack:
            self.stack.pop().__exit__(None, None, None)
        while self.sem_stack:
            self.sem_stack.pop().__exit__(None, None, None)

    def sbuf(self, shape, dt, name=None):
        self.nbuf += 1
        name = (name or f"sb{self.nbuf}") + "_s"
        b = Buf(self._enter(self.nc.sbuf_tensor(name, list(shape), dt)), name)
        self.live.append((len(self.stack), b))
        return b

    def psum(self, shape, dt, name=None):
        self.nbuf += 1
        name = (name or f"ps{self.nbuf}") + "_p"
        b = Buf(self._enter(self.nc.psum_tensor(name, list(shape), dt)), name)
        b.psum = True
        self.live.append((len(self.stack), b))
        return b

    def ring(self, n, shape, dt, name, psum=False):
        mk = self.psum if psum else self.sbuf
        return Ring([mk(shape, dt, f"{name}{i}") for i in range(n)])

    def dram_in(self, name, shape, dt):
        return Buf(self.nc.dram_tensor(name, list(shape), dt, kind="ExternalInput").ap(), name)

    def dram_out(self, name, shape, dt):
        return Buf(self.nc.dram_tensor(name, list(shape), dt, kind="ExternalOutput").ap(), name)

    def dram_tmp(self, name, shape, dt):
        return Buf(self.nc.dram_tensor(name, list(shape), dt, kind="Internal").ap(), name)

    def _need(self, e, dep):
        if dep is None:
            return
        key, val = dep
        if key == e and val > self.cnt[e]:
            return
        if self.known[e].get(key, 0) >= val:
            return
        sem = key[1] if isinstance(key, tuple) else self.sem[key]
        self.eng[e].wait_ge(sem, val)
        self.known[e][key] = val

    def _deps(self, e, reads, writes):
        for b in reads:
            self._need(e, b.w)
        for b in writes:
            self._need(e, b.w)
            for r in b.r:
                self._need(e, r)

    def _mark(self, tag, reads, writes):
        for b in reads:
            b.r.append(tag)
            if len(b.r) > 64:
                last = {}
                for k, v in b.r:
                    last[k] = max(v, last.get(k, 0))
                b.r = list(last.items())
        for b in writes:
            b.w = tag
            b.r = []

    def op(self, e, fn, reads=(), writes=(), inc=True):
        pr = [b for b in reads if b.psum]
        if pr:
            reads = [b for b in reads if not b.psum]
            writes = list(writes) + [b for b in pr if b not in writes]
        self._deps(e, reads, writes)
        ins = fn()
        self.n_inst += 1
        if inc:
            self.cnt[e] += 1
            ins.then_inc(self.sem[e], 1)
            tag = (e, self.cnt[e])
        else:
            tag = (e, self.cnt[e] + 1)
        self._mark(tag, reads, writes)
        return ins

    def dma(self, e, out_ap, in_ap, reads=(), writes=(), sembuf=None, **kw):
        self._deps(e, reads, writes)
        sb = sembuf or (writes[0] if writes else reads[0])
        if sb.dma_sem is None:
            self.nbuf += 1
            sb.dma_sem = self._enter_sem(self.nc.semaphore(f"d{self.nbuf}"))
        ins = self.eng[e].dma_start(out=out_ap, in_=in_ap, **kw)
        sb.dma_cnt += 16
        ins.then_inc(sb.dma_sem, 16)
        self.n_inst += 1
        tag = (("dma", sb.dma_sem, id(sb)), sb.dma_cnt)
        self._mark(tag, reads, writes)
        return ins

    def scope_begin(self):
        return len(self.stack)

    def scope_end(self, mark):
        dead = [b for d, b in self.live if d > mark]
        self.live = [(d, b) for d, b in self.live if d <= mark]
        self.barrier(dead)
        while len(self.stack) > mark:
            self.stack.pop().__exit__(None, None, None)

    def barrier(self, bufs=()):
        for e in ("pe", "act", "dve", "pool", "sp"):
            for f in ("pe", "act", "dve", "pool"):
                if f != e and self.cnt[f] > 0:
                    self._need(e, (f, self.cnt[f]))
            self.wait_all(e, bufs)

    def wait_all(self, e, bufs):
        for b in bufs:
            self._need(e, b.w)
            for r in b.r:
                self._need(e, r)

    def mm(self, out_ap, pairs, reads, writes, start=True, stop=True):
        n = len(pairs)
        for i, (l, r) in enumerate(pairs):
            self.op("pe", lambda l=l, r=r, i=i: self.nc.tensor.matmul(
                out_ap, l, r, start=(start and i == 0), stop=(stop and i == n - 1)),
                reads=reads, writes=writes, inc=(i == n - 1))


def new_nc():
    return bass.Bass("TRN2", target_bir_lowering=False)


def run(nc, in_maps):
    res = run_bass_kernel_spmd(nc, in_maps, core_ids=list(range(NCORES)))
    return res.results


NCOND = 14336
CW = NCOND // NCORES


def build_cond():
    nc = new_nc()
    fw = FW(nc)
    cT = fw.dram_in("cT", [128, 8, 2], F32)
    w = fw.dram_in("w", [D_MODEL, CW], F32)
    b = fw.dram_in("b", [1, CW], F32)
    o = fw.dram_out("cond", [2, CW], F32)
    c_sb = fw.sbuf([128, 8, 2], F32, "c_sb")
    cs = fw.sbuf([128, 8, 2], F32, "cs")
    w_sb = fw.sbuf([128, 8, CW], F32, "w_sb")
    b_sb = fw.sbuf([2, CW], F32, "b_sb")
    o_sb = fw.sbuf([2, CW], F32, "o_sb")
    ps = fw.ring(4, [2, 448], F32, "ps", psum=True)
    fw.dma("sp", c_sb[:], cT[:], writes=[c_sb])
    for ch in range(8):
        fw.dma("sp" if ch % 2 == 0 else "act", w_sb[:, ch, :], w[ch * 128:(ch + 1) * 128, :], writes=[w_sb])
    for r in range(2):
        fw.dma("sp", b_sb[r:r + 1, :], b[:], writes=[b_sb])
    fw.op("act", lambda: nc.scalar.activation(cs[:], c_sb[:], AF.Silu), reads=[c_sb], writes=[cs])
    for j in range(4):
        p = ps.next()
        fw.mm(p[:], [(cs[:, ch, :], w_sb[:, ch, j * 448:(j + 1) * 448]) for ch in range(8)],
              reads=[cs, w_sb], writes=[p])
        fw.op("dve", lambda p=p, j=j: nc.vector.tensor_tensor(
            o_sb[:, j * 448:(j + 1) * 448], p[:], b_sb[:, j * 448:(j + 1) * 448], ALU.add),
            reads=[p, b_sb], writes=[o_sb])
    fw.dma("sp", o[:], o_sb[:], reads=[o_sb], writes=[o])
    fw.wait_all("sp", [o])
    fw.close()
    return nc


def run_cond(inp):
    c = inp["c"]
    cT = np.ascontiguousarray(c.reshape(2, 8, 128).transpose(2, 1, 0))
    w_ada, b_ada = inp["w_ada"], inp["b_ada"]
    maps = []
    for i in range(NCORES):
        maps.append({"cT": cT, "w": np.ascontiguousarray(w_ada[:, i * CW:(i + 1) * CW]),
                     "b": np.ascontiguousarray(b_ada[None, i * CW:(i + 1) * CW])})
    res = run(build_cond(), maps)
    return np.concatenate([r["cond"] for r in res], axis=1)


def mod_off(layer, sub, which):
    return ((layer * 2 + sub) * 3 + which) * D_MODEL


def fm(v):
    return np.ascontiguousarray(v.reshape(-1, 128).T)


DH = 512
GT = 512
NG = SEQ // GT


def build_mlstm_front(SEQ=SEQ):
    NG = SEQ // GT
    nc = new_nc()
    fw = FW(nc)
    xT = fw.dram_in("xT", [D_MODEL, SEQ], F32)
    modp = fw.dram_in("modp", [128, 8, 2], F32)
    w_in = fw.dram_in("w_in", [D_MODEL, 2 * DH], F32)
    convp = fw.dram_in("convp", [128, 4, 6], F32)
    wq = fw.dram_in("wq", [DH, DH], F32)
    wk = fw.dram_in("wk", [DH, DH], F32)
    wv = fw.dram_in("wv", [DH, DH], F32)
    wif = fw.dram_in("wif", [DH, 8], F32)
    qT_o = fw.dram_out("qT", [DH, SEQ], BF16)
    kT_o = fw.dram_out("kT", [DH, SEQ], BF16)
    k_o = fw.dram_out("k", [SEQ, DH], BF16)
    v_o = fw.dram_out("v", [SEQ, DH], BF16)
    sxc_o = fw.dram_out("sxcT", [DH, SEQ], BF16)
    sz_o = fw.dram_out("szT", [DH, SEQ], BF16)
    gp_o = fw.dram_out("gp", [8, SEQ], F32)

    modp_sb = fw.sbuf([128, 8, 2], F32, "modp_sb")
    sc1 = fw.sbuf([128, 8], F32, "sc1")
    convp_sb = fw.sbuf([128, 4, 6], F32, "convp_sb")
    w_in_sb = fw.sbuf([128, 8, 2 * DH], BF16, "w_in_sb")
    wq_sb = fw.sbuf([128, 4, DH], BF16, "wq_sb")
    wk_sb = fw.sbuf([128, 4, DH], BF16, "wk_sb")
    wv_sb = fw.sbuf([128, 4, DH], BF16, "wv_sb")
    wif_sb = fw.sbuf([128, 4, 8], BF16, "wif_sb")
    fw.dma("sp", modp_sb[:], modp[:], writes=[modp_sb])
    fw.dma("sp", convp_sb[:], convp[:], writes=[convp_sb])
    for ch in range(8):
        fw.dma("pool", w_in_sb[:, ch, :], w_in[ch * 128:(ch + 1) * 128, :], writes=[w_in_sb])
    for ch in range(4):
        fw.dma("pool", wq_sb[:, ch, :], wq[ch * 128:(ch + 1) * 128, :], writes=[wq_sb])
        fw.dma("pool", wk_sb[:, ch, :], wk[ch * 128:(ch + 1) * 128, :], writes=[wk_sb])
        fw.dma("pool", wv_sb[:, ch, :], wv[ch * 128:(ch + 1) * 128, :], writes=[wv_sb])
        fw.dma("pool", wif_sb[:, ch, :], wif[ch * 128:(ch + 1) * 128, :], writes=[wif_sb])
    fw.op("dve", lambda: nc.vector.tensor_scalar_add(sc1[:], modp_sb[:, :, 1], 1.0), reads=[modp_sb], writes=[sc1])

    x_r = fw.ring(2, [128, 8, GT], F32, "x_g")
    h_r = fw.ring(2, [128, 8, GT], BF16, "h_g")
    xm_r = fw.ring(2, [128, 4, GT + 4], BF16, "xm_g")
    sz_r = fw.ring(2, [128, 4, GT], BF16, "sz_g")
    acc_r = fw.ring(2, [128, GT], F32, "acc")
    xc_r = fw.ring(2, [128, 4, GT], BF16, "xc_g")
    sxc_r = fw.ring(2, [128, 4, GT], BF16, "sxc_g")
    q_r = fw.ring(2, [128, 4, GT], BF16, "q_g")
    kT_r = fw.ring(2, [128, 4, GT], BF16, "kT_g")
    k_r = fw.ring(2, [128, 4, DH], BF16, "k_g")
    v_r = fw.ring(2, [128, 4, DH], BF16, "v_g")
    gp_r = fw.ring(2, [8, GT], F32, "gp_g")
    ps = fw.ring(7, [128, 512], F32, "ps", psum=True)
    xT_v = xT[:].rearrange("(c p) t -> p c t", p=128)
    xm_prev = None
    xg_next = x_r.next()
    fw.dma("sp", xg_next[:], xT_v[:, :, 0:GT], writes=[xg_next])
    for g in range(NG):
        ts = slice(g * GT, (g + 1) * GT)
        xg = xg_next
        if g + 1 < NG:
            xg_next = x_r.next()
            fw.dma("sp", xg_next[:], xT_v[:, :, (g + 1) * GT:(g + 2) * GT], writes=[xg_next])
        hg = h_r.next()
        for ch in range(8):
            fw.op("dve", lambda ch=ch: nc.vector.tensor_scalar(
                hg[:, ch, :], xg[:, ch, :], sc1[:, ch:ch + 1], modp_sb[:, ch, 0:1], ALU.mult, ALU.add),
                reads=[xg, sc1, modp_sb], writes=[hg])
        xm = xm_r.next()
        sz = sz_r.next()
        if xm_prev is None:
            fw.op("pool", lambda: nc.gpsimd.memset(xm[:, :, 0:4], 0.0), writes=[xm])
        else:
            fw.op("pool", lambda xp=xm_prev: nc.gpsimd.tensor_copy(xm[:, :, 0:4], xp[:, :, GT:GT + 4]),
                  reads=[xm_prev], writes=[xm])
        for m in range(4):
            p = ps.next()
            fw.mm(p[:], [(w_in_sb[:, kk, m * 128:(m + 1) * 128], hg[:, kk, :]) for kk in range(8)],
                  reads=[w_in_sb, hg], writes=[p])
            fw.op("dve", lambda p=p, m=m: nc.vector.tensor_copy(xm[:, m, 4:4 + GT], p[:]), reads=[p], writes=[xm])
        for m in range(4):
            p = ps.next()
            fw.mm(p[:], [(w_in_sb[:, kk, DH + m * 128:DH + (m + 1) * 128], hg[:, kk, :]) for kk in range(8)],
                  reads=[w_in_sb, hg], writes=[p])
            fw.op("act", lambda p=p, m=m: nc.scalar.activation(sz[:, m, :], p[:], AF.Silu), reads=[p], writes=[sz])
        fw.dma("sp", sz_o[:].rearrange("(c p) t -> p c t", p=128)[:, :, ts], sz[:], reads=[sz], writes=[sz_o])
        xc = xc_r.next()
        sxc = sxc_r.next()
        for m in range(4):
            acc = acc_r.next()
            fw.op("dve", lambda m=m, acc=acc: nc.vector.tensor_scalar(
                acc[:], xm[:, m, 4:4 + GT], convp_sb[:, m, 3:4], convp_sb[:, m, 4:5], ALU.mult, ALU.add),
                reads=[xm, convp_sb], writes=[acc])
            for j in range(3):
                fw.op("dve", lambda m=m, acc=acc, j=j: nc.vector.scalar_tensor_tensor(
                    acc[:], xm[:, m, 1 + j:1 + j + GT], convp_sb[:, m, j:j + 1], acc[:], ALU.mult, ALU.add),
                    reads=[xm, convp_sb, acc], writes=[acc])
            fw.op("act", lambda m=m, acc=acc: nc.scalar.activation(xc[:, m, :], acc[:], AF.Silu),
                  reads=[acc], writes=[xc])
            fw.op("pool", lambda m=m: nc.gpsimd.tensor_scalar(
                sxc[:, m, :], xc[:, m, :], convp_sb[:, m, 5:6], None, ALU.mult),
                reads=[xc, convp_sb], writes=[sxc])
        fw.dma("sp", sxc_o[:].rearrange("(c p) t -> p c t", p=128)[:, :, ts], sxc[:], reads=[sxc], writes=[sxc_o])
        qg = q_r.next()
        kTg = kT_r.next()
        for m in range(4):
            p = ps.next()
            fw.mm(p[:], [(wq_sb[:, kk, m * 128:(m + 1) * 128], xc[:, kk, :]) for kk in range(4)],
                  reads=[wq_sb, xc], writes=[p])
            fw.op("act", lambda p=p, m=m: nc.scalar.copy(qg[:, m, :], p[:]), reads=[p], writes=[qg])
        for m in range(4):
            p = ps.next()
            fw.mm(p[:], [(wk_sb[:, kk, m * 128:(m + 1) * 128], xc[:, kk, :]) for kk in range(4)],
                  reads=[wk_sb, xc], writes=[p])
            fw.op("act", lambda p=p, m=m: nc.scalar.mul(kTg[:, m, :], p[:], DH ** -0.5), reads=[p], writes=[kTg])
        fw.dma("act", qT_o[:].rearrange("(c p) t -> p c t", p=128)[:, :, ts], qg[:], reads=[qg], writes=[qT_o])
        fw.dma("act", kT_o[:].rearrange("(c p) t -> p c t", p=128)[:, :, ts], kTg[:], reads=[kTg], writes=[kT_o])
        kg = k_r.next()
        vg = v_r.next()
        for tt in range(4):
            p = ps.next()
            fw.mm(p[:], [(xc[:, kk, tt * 128:(tt + 1) * 128], wk_sb[:, kk, :]) for kk in range(4)],
                  reads=[wk_sb, xc], writes=[p])
            fw.op("act", lambda p=p, tt=tt: nc.scalar.mul(kg[:, tt, :], p[:], DH ** -0.5), reads=[p], writes=[kg])
            p = ps.next()
            fw.mm(p[:], [(xm[:, kk, 4 + tt * 128:4 + (tt + 1) * 128], wv_sb[:, kk, :]) for kk in range(4)],
                  reads=[wv_sb, xm], writes=[p])
            fw.op("dve", lambda p=p, tt=tt: nc.vector.tensor_copy(vg[:, tt, :], p[:]), reads=[p], writes=[vg])
        fw.dma("sp", k_o[ts, :].rearrange("(tt p) f -> p tt f", p=128), kg[:], reads=[kg], writes=[k_o])
        fw.dma("sp", v_o[ts, :].rearrange("(tt p) f -> p tt f", p=128), vg[:], reads=[vg], writes=[v_o])
        p = ps.next()
        fw.mm(p[0:8, :], [(wif_sb[:, kk, :], xc[:, kk, :]) for kk in range(4)], reads=[wif_sb, xc], writes=[p])
        gpg = gp_r.next()
        fw.op("dve", lambda p=p: nc.vector.tensor_copy(gpg[:], p[0:8, :]), reads=[p], writes=[gpg])
        fw.dma("sp", gp_o[:, ts], gpg[:], reads=[gpg], writes=[gp_o])
        xm_prev = xm
    fw.wait_all("sp", [qT_o, kT_o, k_o, v_o, sxc_o, sz_o, gp_o])
    fw.wait_all("act", [qT_o, kT_o, k_o, v_o, sxc_o, sz_o, gp_o])
    fw.close()
    return nc


def run_mlstm_front(inp, cond):
    maps = []
    for core in range(NCORES):
        b, hd = divmod(core, 4)
        xT = np.ascontiguousarray(inp["x"][b].T)
        shift = cond[b, mod_off(0, 0, 0):mod_off(0, 0, 0) + D_MODEL]
        scale = cond[b, mod_off(0, 0, 1):mod_off(0, 0, 1) + D_MODEL]
        modp = np.ascontiguousarray(np.stack([fm(shift), fm(scale)], axis=-1))
        w_in = inp["a_w_in"][0]
        w_in_c = np.ascontiguousarray(np.concatenate(
            [w_in[:, hd * DH:(hd + 1) * DH], w_in[:, 2048 + hd * DH:2048 + (hd + 1) * DH]], axis=1))
        sl = slice(hd * DH, (hd + 1) * DH)
        cw = inp["a_conv_w"][0][:, sl]
        rows = [cw[j] for j in range(4)] + [inp["a_conv_b"][0][sl], inp["a_skip"][0][sl]]
        convp = np.ascontiguousarray(np.stack([r.reshape(4, 128).T for r in rows], axis=-1))
        maps.append({"xT": xT, "modp": modp, "w_in": w_in_c, "convp": convp,
                     "wq": np.ascontiguousarray(inp["a_wq"][0][hd]), "wk": np.ascontiguousarray(inp["a_wk"][0][hd]),
                     "wv": np.ascontiguousarray(inp["a_wv"][0][hd]),
                     "wif": np.ascontiguousarray(inp["a_w_if"][0][sl, :])})
    return run(build_mlstm_front(), maps)


LCH = 128
NCH = SEQ // LCH


def build_mlstm_core(SEQ=SEQ):
    NCH = SEQ // LCH
    nc = new_nc()
    fw = FW(nc)
    qT = fw.dram_in("qT", [DH, SEQ], BF16)
    kT = fw.dram_in("kT", [DH, SEQ], BF16)
    k_i = fw.dram_in("k", [SEQ, DH], BF16)
    v_i = fw.dram_in("v", [SEQ, DH], BF16)
    sxc_i = fw.dram_in("sxcT", [DH, SEQ], BF16)
    sz_i = fw.dram_in("szT", [DH, SEQ], BF16)
    gpi = fw.dram_in("gpi", [4, SEQ], F32)
    gpf = fw.dram_in("gpf", [4, SEQ], F32)
    bif = fw.dram_in("bif", [1, 2], F32)
    gng = fw.dram_in("gng", [128, 4], F32)
    cst = fw.dram_in("cst", [128, 256], F32)
    hg_o = fw.dram_out("hgT", [DH, SEQ], BF16)

    cst_sb = fw.sbuf([128, 256], F32, "cst_sb")
    ident_b = fw.sbuf([128, 128], BF16, "ident_b")
    ones_f = fw.sbuf([128, 128], F32, "ones_f")
    ones_b = fw.sbuf([128, 1], BF16, "ones_b")
    gng_sb = fw.sbuf([128, 4], F32, "gng_sb")
    bif_sb = fw.sbuf([1, 2], F32, "bif_sb")
    cols = fw.sbuf([128, 3, NCH], F32, "cols")
    ulast = fw.sbuf([128, NCH], F32, "ulast")
    u0 = fw.sbuf([128, NCH], F32, "u0")
    wint = fw.sbuf([128, NCH], F32, "wint")
    emt = fw.sbuf([128, NCH], F32, "emt")
    wsc = fw.sbuf([128, NCH], F32, "wsc")
    dec = fw.sbuf([128, NCH], F32, "dec")
    ident_f = cst_sb
    fw.dma("sp", cst_sb[:], cst[:], writes=[cst_sb])
    fw.dma("pool", ident_b[:], cst[:, 0:128], writes=[ident_b])
    fw.dma("sp", gng_sb[:], gng[:], writes=[gng_sb])
    fw.dma("sp", bif_sb[:], bif[:], writes=[bif_sb])
    fw.op("dve", lambda: nc.vector.memset(ones_f[:], 1.0), writes=[ones_f])
    fw.op("dve", lambda: nc.vector.memset(ones_b[:], 1.0), writes=[ones_b])

    mark = fw.scope_begin()
    g4 = fw.sbuf([4, SEQ], F32, "g4")
    irow = fw.sbuf([1, SEQ], F32, "irow")
    frow = fw.sbuf([1, SEQ], F32, "frow")
    trow = fw.sbuf([1, SEQ], F32, "trow")
    brow = fw.sbuf([1, SEQ], F32, "brow")
    gps = fw.ring(4, [128, 512], F32, "gps", psum=True)
    for src, dst, bi in ((gpi, irow, 0), (gpf, frow, 1)):
        fw.dma("sp", g4[:], src[:], writes=[g4])
        for j in range(SEQ // 512):
            p = gps.next()
            fw.mm(p[0:1, :], [(ones_f[0:4, 0:1], g4[:, j * 512:(j + 1) * 512])], reads=[ones_f, g4], writes=[p])
            fw.op("act", lambda p=p, j=j, dst=dst, bi=bi: nc.scalar.activation(
                dst[:, j * 512:(j + 1) * 512], p[0:1, :], AF.Identity, bias=bif_sb[:, bi:bi + 1]),
                reads=[p, bif_sb], writes=[dst])
    fw.op("act", lambda: nc.scalar.activation(trow[:], frow[:], AF.Abs), reads=[frow], writes=[trow])
    fw.op("act", lambda: nc.scalar.activation(trow[:], trow[:], AF.Exp, scale=-1.0), reads=[trow], writes=[trow])
    fw.op("act", lambda: nc.scalar.activation(trow[:], trow[:], AF.Ln, bias=1.0), reads=[trow], writes=[trow])
    fw.op("dve", lambda: nc.vector.scalar_tensor_tensor(frow[:], frow[:], 0.0, trow[:], ALU.min, ALU.subtract),
          reads=[frow, trow], writes=[frow])
    one_bc = ones_f[0:1, 0:1].to_broadcast([1, SEQ])
    fw.op("dve", lambda: nc.vector.tensor_tensor_scan(brow[:], one_bc, frow[:], 0.0, ALU.mult, ALU.add),
          reads=[ones_f, frow], writes=[brow])
    fw.op("dve", lambda: nc.vector.tensor_sub(irow[:], irow[:], brow[:]), reads=[irow, brow], writes=[irow])
    fw.op("dve", lambda: nc.vector.tensor_tensor_scan(trow[:], one_bc, irow[:], 0.0, ALU.mult, ALU.max),
          reads=[ones_f, irow], writes=[trow])
    fw.op("dve", lambda: nc.vector.tensor_add(brow[:], brow[:], trow[:]), reads=[brow, trow], writes=[brow])
    cp = gps.next()
    for ai, row in enumerate((irow, trow, brow)):
        for c in range(NCH):
            fw.op("pe", lambda ai=ai, row=row, c=c: nc.tensor.transpose(
                cp[:, ai * NCH + c:ai * NCH + c + 1], row[0:1, c * LCH:(c + 1) * LCH], ident_f[0:1, 0:1]),
                reads=[row, cst_sb], writes=[cp], inc=(c == NCH - 1))
    fw.op("dve", lambda: nc.vector.tensor_copy(cols[:].rearrange("p a c -> p (a c)"), cp[:, 0:3 * NCH]),
          reads=[cp], writes=[cols])
    bp = gps.next()
    fw.mm(bp[:, 0:NCH], [(ones_f[0:1, :], trow[0:1, LCH - 1::LCH])], reads=[ones_f, trow], writes=[bp])
    fw.op("dve", lambda: nc.vector.tensor_copy(ulast[:], bp[:, 0:NCH]), reads=[bp], writes=[ulast])
    fw.op("dve", lambda: nc.vector.memset(u0[:, 0:1], 0.0), writes=[u0])
    fw.op("dve", lambda: nc.vector.tensor_copy(u0[:, 1:NCH], ulast[:, 0:NCH - 1]), reads=[ulast], writes=[u0])
    tmpc = fw.sbuf([128, NCH], F32, "tmpc")
    fw.op("dve", lambda: nc.vector.tensor_sub(tmpc[:], u0[:], cols[:, 1, :]), reads=[u0, cols], writes=[tmpc])
    fw.op("act", lambda: nc.scalar.activation(wint[:], tmpc[:], AF.Exp), reads=[tmpc], writes=[wint])
    fw.op("act", lambda: nc.scalar.activation(emt[:], cols[:, 2, :], AF.Exp, scale=-1.0), reads=[cols], writes=[emt])
    fw.op("dve", lambda: nc.vector.tensor_sub(tmpc[:], cols[:, 0, :], ulast[:]), reads=[ulast, cols], writes=[tmpc])
    fw.op("act", lambda: nc.scalar.activation(wsc[:], tmpc[:], AF.Exp), reads=[tmpc], writes=[wsc])
    fw.op("dve", lambda: nc.vector.tensor_sub(tmpc[:], u0[:], ulast[:]), reads=[u0, ulast], writes=[tmpc])
    fw.op("act", lambda: nc.scalar.activation(dec[:], tmpc[:], AF.Exp), reads=[tmpc], writes=[dec])
    fw.scope_end(mark)

    GC = 4
    q_r = fw.ring(2, [128, 4, GC * LCH], BF16, "q_c")
    kT_r = fw.ring(2, [128, 4, GC * LCH], BF16, "kT_c")
    sxc_r = fw.ring(2, [128, 4, GC * LCH], BF16, "sxc_c")
    sz_r = fw.ring(2, [128, 4, GC * LCH], BF16, "sz_c")
    k_r = fw.ring(2, [128, GC, DH], BF16, "k_c")
    v_r = fw.ring(2, [128, GC, DH], BF16, "v_c")
    hg_r = fw.ring(2, [128, 4, GC * LCH], BF16, "hg_c")
    Cm = fw.sbuf([128, 4, DH], F32, "Cm")
    nm = fw.sbuf([128, 4], F32, "nm")
    Cb = fw.sbuf([128, 4, DH], BF16, "Cb")
    nb = fw.sbuf([128, 4], BF16, "nb")
    diag = fw.ring(2, [128, 128], F32, "diag")
    DT = fw.ring(2, [128, 128], F32, "DT")
    swT = fw.ring(2, [128, 128], BF16, "swT")
    kw = fw.ring(2, [128, DH], BF16, "kw")
    dsb = fw.ring(2, [128, 8], F32, "dsb")
    inter_sb = fw.ring(2, [128, DH], F32, "inter_sb")
    h_sb = fw.ring(2, [128, DH], F32, "h_sb")
    hn_sb = fw.ring(2, [128, DH], BF16, "hn_sb")
    st = fw.ring(2, [128, 8], F32, "st")
    a_sb = fw.ring(2, [128, 4, LCH], F32, "a_sb")
    pA = fw.psum([128, 512], F32, "pA")
    pB = fw.psum([128, 512], F32, "pB")
    pC = fw.psum([128, 512], F32, "pC")
    pT = fw.psum([128, 4, LCH], BF16, "pT")
    pD = [fw.psum([128, 512], F32, f"pD{m}") for m in range(2)]
    pN = fw.psum([128, 8], F32, "pN")
    fw.op("dve", lambda: nc.vector.memset(Cm[:], 0.0), writes=[Cm])
    fw.op("dve", lambda: nc.vector.memset(nm[:], 0.0), writes=[nm])
    fw.op("pool", lambda: nc.gpsimd.memset(Cb[:], 0.0), writes=[Cb])
    fw.op("pool", lambda: nc.gpsimd.memset(nb[:], 0.0), writes=[nb])
    fmv = lambda ap: ap[:].rearrange("(c p) t -> p c t", p=128)
    def load_group(g):
        ts = slice(g * GC * LCH, (g + 1) * GC * LCH)
        bufs = (q_r.next(), kT_r.next(), sxc_r.next(), sz_r.next(), k_r.next(), v_r.next())
        qg, kTg, sxg, szg, kg, vg = bufs
        fw.dma("sp", qg[:], fmv(qT)[:, :, ts], writes=[qg])
        fw.dma("sp", kTg[:], fmv(kT)[:, :, ts], writes=[kTg])
        fw.dma("sp", kg[:], k_i[ts, :].rearrange("(tt p) f -> p tt f", p=128), writes=[kg])
        fw.dma("sp", vg[:], v_i[ts, :].rearrange("(tt p) f -> p tt f", p=128), writes=[vg])
        fw.dma("sp", sxg[:], fmv(sxc_i)[:, :, ts], writes=[sxg])
        fw.dma("sp", szg[:], fmv(sz_i)[:, :, ts], writes=[szg])
        return bufs

    nxt = load_group(0)
    for g in range(NCH // GC):
        ts = slice(g * GC * LCH, (g + 1) * GC * LCH)
        qg, kTg, sxg, szg, kg, vg = nxt
        hgg = hg_r.next()
        if g + 1 < NCH // GC:
            nxt = load_group(g + 1)
        for cc in range(GC):
            c = g * GC + cc
            cs = slice(cc * LCH, (cc + 1) * LCH)
            fw.mm(pC[:], [(qg[:, m, cs], Cb[:, m, :]) for m in range(4)], reads=[qg, Cb], writes=[pC])
            fw.mm(pA[:, 257:258], [(qg[:, m, cs], nb[:, m:m + 1]) for m in range(4)], reads=[qg, nb], writes=[pA])
            kwt = kw.next()
            fw.op("pool", lambda kwt=kwt, c=c, cc=cc: nc.gpsimd.tensor_scalar(
                kwt[:], kg[:, cc, :], wsc[:, c:c + 1], None, ALU.mult), reads=[kg, wsc], writes=[kwt])
            for m in range(4):
                fw.mm(pN[:, m:m + 1], [(kwt[:, m * 128:(m + 1) * 128], ones_b[:])], reads=[kwt, ones_b], writes=[pN])
            for m in range(4):
                pd = pD[m % 2]
                fw.mm(pd[:], [(kwt[:, m * 128:(m + 1) * 128], vg[:, cc, :])], reads=[kwt, vg], writes=[pd])
                fw.op("dve", lambda m=m, c=c, pd=pd: nc.vector.scalar_tensor_tensor(
                    Cm[:, m, :], Cm[:, m, :], dec[:, c:c + 1], pd[:], ALU.mult, ALU.add),
                    reads=[Cm, dec, pd], writes=[Cm])
            fw.op("dve", lambda c=c: nc.vector.scalar_tensor_tensor(
                nm[:], nm[:], dec[:, c:c + 1], pN[:, 0:4], ALU.mult, ALU.add), reads=[nm, dec, pN], writes=[nm])
            fw.mm(pA[:, 0:128], [(kTg[:, m, cs], qg[:, m, cs]) for m in range(4)], reads=[kTg, qg], writes=[pA])
            dg = diag.next()
            fw.op("dve", lambda dg=dg, c=c: nc.vector.tensor_scalar(
                dg[:], ident_f[:, 0:128], cols[:, 1, c:c + 1], None, ALU.mult), reads=[cst_sb, cols], writes=[dg])
            fw.mm(pA[:, 128:256], [(ones_f[:], dg[:]), (ident_f[:, 0:128], cst_sb[:, 128:256])],
                  reads=[ones_f, dg, cst_sb], writes=[pA])
            dt = DT.next()
            fw.op("act", lambda dt=dt, c=c: nc.scalar.activation(
                dt[:], pA[:, 128:256], AF.Exp, bias=cols[:, 0, c:c + 1], scale=-1.0), reads=[pA, cols], writes=[dt])
            sw = swT.next()
            fw.op("dve", lambda sw=sw, dt=dt: nc.vector.tensor_mul(sw[:], pA[:, 0:128], dt[:]), reads=[pA, dt], writes=[sw])
            fw.mm(pB[:], [(sw[:], vg[:, cc, :])], reads=[sw, vg], writes=[pB])
            fw.mm(pA[:, 256:257], [(sw[:], ones_b[:])], reads=[sw, ones_b], writes=[pA])
            fw.op("act", lambda: nc.scalar.copy(Cb[:], Cm[:]), reads=[Cm], writes=[Cb])
            fw.op("act", lambda: nc.scalar.copy(nb[:], nm[:]), reads=[nm], writes=[nb])
            d = dsb.next()
            fw.op("dve", lambda d=d: nc.vector.tensor_copy(d[:, 0:2], pA[:, 256:258]), reads=[pA], writes=[d])
            fw.op("dve", lambda d=d, c=c: nc.vector.scalar_tensor_tensor(
                d[:, 2:3], d[:, 1:2], wint[:, c:c + 1], d[:, 0:1], ALU.mult, ALU.add), reads=[d, wint], writes=[d])
            fw.op("dve", lambda d=d: nc.vector.tensor_scalar(d[:, 6:7], d[:, 2:3], -1.0, None, ALU.mult), reads=[d], writes=[d])
            fw.op("dve", lambda d=d: nc.vector.tensor_max(d[:, 6:7], d[:, 6:7], d[:, 2:3]), reads=[d], writes=[d])
            fw.op("dve", lambda d=d, c=c: nc.vector.tensor_max(d[:, 3:4], d[:, 6:7], emt[:, c:c + 1]),
                  reads=[d, emt], writes=[d])
            fw.op("dve", lambda d=d: nc.vector.reciprocal(d[:, 4:5], d[:, 3:4]), reads=[d], writes=[d])
            fw.op("dve", lambda d=d, c=c: nc.vector.tensor_mul(d[:, 5:6], d[:, 4:5], wint[:, c:c + 1]),
                  reads=[d, wint], writes=[d])
            isb = inter_sb.next()
            fw.op("act", lambda isb=isb, d=d: nc.scalar.activation(isb[:], pC[:], AF.Copy, scale=d[:, 5:6]),
                  reads=[pC, d], writes=[isb])
            h = h_sb.next()
            fw.op("dve", lambda h=h, d=d, isb=isb: nc.vector.scalar_tensor_tensor(
                h[:], pB[:], d[:, 4:5], isb[:], ALU.mult, ALU.add), reads=[pB, d, isb], writes=[h])
            s = st.next()
            fw.op("dve", lambda s=s, h=h: nc.vector.bn_stats(s[:, 0:6], h[:]), reads=[h], writes=[s])
            fw.op("dve", lambda s=s: nc.vector.bn_aggr(s[:, 6:8], s[:, 0:6]), reads=[s], writes=[s])
            fw.op("act", lambda s=s: nc.scalar.activation(s[:, 7:8], s[:, 7:8], AF.Ln, bias=LN_EPS), reads=[s], writes=[s])
            fw.op("act", lambda s=s: nc.scalar.activation(s[:, 7:8], s[:, 7:8], AF.Exp, scale=-0.5), reads=[s], writes=[s])
            hn = hn_sb.next()
            fw.op("dve", lambda hn=hn, h=h, s=s: nc.vector.tensor_scalar(
                hn[:], h[:], s[:, 6:7], s[:, 7:8], ALU.subtract, ALU.mult), reads=[h, s], writes=[hn])
            for m in range(4):
                fw.op("pe", lambda m=m, hn=hn: nc.tensor.transpose(pT[:, m, :], hn[:, m * 128:(m + 1) * 128], ident_b[:]),
                      reads=[hn, ident_b], writes=[pT], inc=(m == 3))
            a = a_sb.next()
            for m in range(4):
                fw.op("dve", lambda m=m, a=a: nc.vector.scalar_tensor_tensor(
                    a[:, m, :], pT[:, m, :], gng_sb[:, m:m + 1], sxg[:, m, cs], ALU.mult, ALU.add),
                    reads=[pT, gng_sb, sxg], writes=[a])
            fw.op("pool", lambda a=a: nc.gpsimd.tensor_mul(hgg[:, :, cs], a[:], szg[:, :, cs]), reads=[a, szg], writes=[hgg])
        fw.dma("sp", fmv(hg_o)[:, :, ts], hgg[:], reads=[hgg], writes=[hg_o])
    fw.wait_all("sp", [hg_o])
    fw.close()
    return nc


def mlstm_consts():
    cst = np.zeros((128, 256), np.float32)
    cst[:, :128] = np.eye(128, dtype=np.float32)
    sp, t = np.meshgrid(np.arange(128), np.arange(128), indexing="ij")
    cst[:, 128:] = np.where(sp > t, BIG, 0.0)
    return cst


def run_mlstm_core(inp, front):
    maps = []
    cst = mlstm_consts()
    for core in range(NCORES):
        b, hd = divmod(core, 4)
        f = front[core]
        gpi = np.ascontiguousarray(np.stack([front[b * 4 + j]["gp"][hd] for j in range(4)]))
        gpf = np.ascontiguousarray(np.stack([front[b * 4 + j]["gp"][4 + hd] for j in range(4)]))
        b_if = inp["a_b_if"][0]
        maps.append({"qT": f["qT"], "kT": f["kT"], "k": f["k"], "v": f["v"], "sxcT": f["sxcT"], "szT": f["szT"],
                     "gpi": gpi, "gpf": gpf, "bif": np.array([[b_if[hd], b_if[4 + hd]]], np.float32),
                     "gng": np.ascontiguousarray(inp["a_gn_g"][0][hd * DH:(hd + 1) * DH].reshape(4, 128).T),
                     "cst": cst})
    return run(build_mlstm_core(), maps)


NT = 16
NE = 16
DFF = 512


def load_rows(fw, nc, rows_d, idxs, name, plus1=()):
    t = fw.sbuf([128, len(idxs), D_MODEL], F32, name)
    for j, i in enumerate(idxs):
        fw.dma("sp" if j % 2 == 0 else "act", t[:, j, :], rows_d[i:i + 1, :].to_broadcast([128, D_MODEL]), writes=[t])
    for j, i in enumerate(idxs):
        if i in plus1:
            fw.op("pool", lambda j=j: nc.gpsimd.tensor_scalar_add(t[:, j, :], t[:, j, :], 1.0), reads=[t], writes=[t])
    return t


def ln_tile(fw, nc, r, st, rows, ig, ib, x_out):
    fw.op("dve", lambda: nc.vector.bn_stats(st[:, 0:6], r[:, 0:512]), reads=[r], writes=[st])
    fw.op("dve", lambda: nc.vector.bn_stats(st[:, 6:12], r[:, 512:1024]), reads=[r], writes=[st])
    fw.op("dve", lambda: nc.vector.bn_aggr(st[:, 12:14], st[:, 0:12]), reads=[st], writes=[st])
    fw.op("act", lambda: nc.scalar.activation(st[:, 13:14], st[:, 13:14], AF.Ln, bias=LN_EPS), reads=[st], writes=[st])
    fw.op("act", lambda: nc.scalar.activation(st[:, 13:14], st[:, 13:14], AF.Exp, scale=-0.5), reads=[st], writes=[st])
    fw.op("dve", lambda: nc.vector.tensor_scalar(r[:], r[:], st[:, 12:13], st[:, 13:14], ALU.subtract, ALU.mult),
          reads=[r, st], writes=[r])
    fw.op("pool", lambda: nc.gpsimd.tensor_mul(r[:], r[:], rows[:, ig, :]), reads=[r, rows], writes=[r])
    fw.op("dve", lambda: nc.vector.tensor_add(x_out[:], r[:], rows[:, ib, :]), reads=[r, rows], writes=[x_out])


def mod_tile(fw, nc, x_in, rows, isc, ish, h_out):
    fw.op("pool", lambda: nc.gpsimd.tensor_mul(h_out[:], x_in[:], rows[:, isc, :]), reads=[x_in, rows], writes=[h_out])
    fw.op("dve", lambda: nc.vector.tensor_add(h_out[:], h_out[:], rows[:, ish, :]), reads=[h_out, rows], writes=[h_out])


def transpose_tile(fw, nc, h32, ident, ps2, outs):
    for k in range(8):
        fw.op("pe", lambda k=k: nc.tensor.transpose(ps2[:, k * 128:(k + 1) * 128], h32[:, k * 128:(k + 1) * 128],
                                                    ident[:, 0:128]),
              reads=[h32, ident], writes=[ps2], inc=(k == 7))
    src = ps2[:].rearrange("p (k t) -> p k t", k=8)
    for e, dst, dbuf in outs:
        if e == "act":
            fw.op("act", lambda dst=dst: nc.scalar.copy(dst, src), reads=[ps2], writes=[dbuf])
        else:
            fw.op("dve", lambda dst=dst: nc.vector.tensor_copy(dst, src), reads=[ps2], writes=[dbuf])


def router_tile(fw, nc, hT32, wr_sb, brow, psl, rt, cw_out):
    fw.mm(psl[:, 0:16], [(hT32[:, k, :], wr_sb[:, k, :]) for k in range(8)], reads=[hT32, wr_sb], writes=[psl])
    V = nc.vector
    o = lambda fn, reads=(), writes=(): fw.op("dve", fn, reads=list(reads) + [rt], writes=list(writes) + [rt])
    o(lambda: V.reduce_max(rt[:, 0:1], psl[:, 0:16], AX.X), reads=[psl])
    o(lambda: V.tensor_scalar(rt[:, 1:2], rt[:, 0:1], -1.0, None, ALU.mult))
    fw.op("act", lambda: nc.scalar.activation(rt[:, 32:48], psl[:, 0:16], AF.Exp, bias=rt[:, 1:2], accum_out=rt[:, 2:3]),
          reads=[psl, rt], writes=[rt])
    o(lambda: V.reciprocal(rt[:, 3:4], rt[:, 2:3]))
    o(lambda: V.tensor_scalar(rt[:, 32:48], rt[:, 32:48], rt[:, 3:4], None, ALU.mult))
    o(lambda: V.tensor_add(rt[:, 48:64], rt[:, 32:48], brow[:]), reads=[brow])
    g3 = lambda a: rt[:, a:a + 16].rearrange("p (g e) -> p g e", g=4)
    bc4 = lambda a: rt[:, a:a + 4].unsqueeze(2).to_broadcast([128, 4, 4])
    o(lambda: V.tensor_reduce(rt[:, 4:8], g3(48), AX.X, ALU.max))
    o(lambda: V.tensor_tensor(g3(64), g3(48), bc4(4), ALU.is_equal))
    o(lambda: V.scalar_tensor_tensor(rt[:, 64:80], rt[:, 64:80], -BIG, rt[:, 48:64], ALU.mult, ALU.add))
    o(lambda: V.tensor_reduce(rt[:, 8:12], g3(64), AX.X, ALU.max))
    o(lambda: V.tensor_add(rt[:, 8:12], rt[:, 8:12], rt[:, 4:8]))
    o(lambda: V.reduce_max(rt[:, 12:13], rt[:, 8:12], AX.X))
    o(lambda: V.tensor_scalar(rt[:, 13:17], rt[:, 8:12], rt[:, 12:13], None, ALU.is_equal))
    o(lambda: V.tensor_scalar(rt[:, 13:17], rt[:, 13:17], -1.0, BIG, ALU.add, ALU.mult))
    o(lambda: V.tensor_tensor(g3(64), g3(48), bc4(13), ALU.add))
    o(lambda: V.reduce_max(rt[:, 17:18], rt[:, 64:80], AX.X))
    o(lambda: V.tensor_scalar(rt[:, 80:96], rt[:, 64:80], rt[:, 17:18], None, ALU.is_equal))
    o(lambda: V.scalar_tensor_tensor(rt[:, 96:112], rt[:, 80:96], -BIG, rt[:, 64:80], ALU.mult, ALU.add))
    o(lambda: V.reduce_max(rt[:, 18:19], rt[:, 96:112], AX.X))
    o(lambda: V.tensor_scalar(rt[:, 96:112], rt[:, 96:112], rt[:, 18:19], None, ALU.is_equal))
    o(lambda: V.tensor_add(rt[:, 80:96], rt[:, 80:96], rt[:, 96:112]))
    o(lambda: V.tensor_mul(rt[:, 112:128], rt[:, 32:48], rt[:, 80:96]))
    o(lambda: V.reduce_sum(rt[:, 19:20], rt[:, 112:128], AX.X))
    o(lambda: V.reciprocal(rt[:, 20:21], rt[:, 19:20]))
    fw.op("dve", lambda: V.tensor_scalar(cw_out, rt[:, 112:128], rt[:, 20:21], None, ALU.mult), reads=[rt], writes=[rt])


def moe_dense(fw, nc, hT, cw, cw_buf, wg_d, wu_d, wd_d, yacc):
    mark = fw.scope_begin()
    wg_r = fw.ring(2, [128, 8, DFF], BF16, "wg")
    wu_r = fw.ring(2, [128, 8, DFF], BF16, "wu")
    wd_r = fw.ring(2, [128, 4, D_MODEL], BF16, "wd")
    sg_r = fw.ring(2, [128, 512], F32, "sg")
    aT_r = fw.ring(2, [128, 4, 512], BF16, "aT")
    psg = fw.ring(2, [128, 512], F32, "psg", psum=True)
    psu = fw.ring(2, [128, 512], F32, "psu", psum=True)
    psd = fw.ring(3, [128, 512], F32, "psd", psum=True)
    for e in range(NE):
        wg, wu, wd = wg_r.next(), wu_r.next(), wd_r.next()
        fw.dma("pool", wg[:], wg_d[e].rearrange("(k p) f -> p k f", p=128), writes=[wg])
        fw.dma("pool", wu[:], wu_d[e].rearrange("(k p) f -> p k f", p=128), writes=[wu])
        fw.dma("pool", wd[:], wd_d[e].rearrange("(k p) f -> p k f", p=128), writes=[wd])
        for g in range(NT // 4):
            gs = slice(g * 512, (g + 1) * 512)
            aT = aT_r.next()
            for m in range(4):
                pg, pu = psg.next(), psu.next()
                fw.mm(pg[:], [(wg[:, k, m * 128:(m + 1) * 128], hT[:, k, gs]) for k in range(8)], reads=[wg, hT], writes=[pg])
                fw.mm(pu[:], [(wu[:, k, m * 128:(m + 1) * 128], hT[:, k, gs]) for k in range(8)], reads=[wu, hT], writes=[pu])
                sg = sg_r.next()
                fw.op("act", lambda sg=sg, pg=pg: nc.scalar.activation(sg[:], pg[:], AF.Silu), reads=[pg], writes=[sg])
                fw.op("dve", lambda sg=sg, pu=pu, m=m, aT=aT: nc.vector.tensor_mul(aT[:, m, :], pu[:], sg[:]),
                      reads=[pu, sg], writes=[aT])
            for tt in range(4):
                T = g * 4 + tt
                for half in range(2):
                    pd = psd.next()
                    fw.mm(pd[:], [(aT[:, m, tt * 128:(tt + 1) * 128], wd[:, m, half * 512:(half + 1) * 512]) for m in range(4)],
                          reads=[aT, wd], writes=[pd])
                    ysl = yacc[:, T, half * 512:(half + 1) * 512]
                    if e == 0:
                        fw.op("dve", lambda pd=pd, ysl=ysl, T=T, e=e: nc.vector.tensor_scalar(
                            ysl, pd[:], cw[:, T, e:e + 1], None, ALU.mult), reads=[pd, cw_buf], writes=[yacc])
                    else:
                        fw.op("dve", lambda pd=pd, ysl=ysl, T=T, e=e: nc.vector.scalar_tensor_tensor(
                            ysl, pd[:], cw[:, T, e:e + 1], ysl, ALU.mult, ALU.add), reads=[pd, cw_buf, yacc], writes=[yacc])
    fw.scope_end(mark)


def mix_ln_phase(fw, nc, y_fn, x_src, x1_d, rows, hT, cw, cw_buf, wr_sb, brow, ident_f, dbg=None, pst=None, psy_n=2):
    x_r = fw.ring(2, [128, D_MODEL], F32, "xa")
    r_r = fw.ring(2, [128, D_MODEL], F32, "ra")
    x1_r = fw.ring(2, [128, D_MODEL], F32, "x1a")
    h_r = fw.ring(2, [128, D_MODEL], F32, "ha")
    hT32_r = fw.ring(2, [128, 8, 128], F32, "hT32")
    st_r = fw.ring(2, [128, 16], F32, "sta")
    rt_r = fw.ring(2, [128, 128], F32, "rt")
    psy = fw.ring(psy_n, [128, D_MODEL], F32, "psy", psum=True)
    if pst is None:
        pst = fw.ring(1, [128, D_MODEL], F32, "pst", psum=True)
    psl = fw.ring(2, [128, 512], F32, "psl", psum=True)
    xt_next = x_r.next()
    fw.dma("sp", xt_next[:], x_src[0:128, :], writes=[xt_next])
    for T in range(NT if dbg is None else 1):
        xt = xt_next
        if T + 1 < NT:
            xt_next = x_r.next()
            fw.dma("sp", xt_next[:], x_src[(T + 1) * 128:(T + 2) * 128, :], writes=[xt_next])
        py = psy.next()
        if dbg == "A0":
            return
        y_fn(T, py)
        r = r_r.next()
        fw.op("dve", lambda r=r, py=py: nc.vector.tensor_mul(r[:], py[:], rows[:, 0, :]), reads=[py, rows], writes=[r])
        fw.op("dve", lambda r=r, xt=xt: nc.vector.scalar_tensor_tensor(r[:], xt[:], ALPHA, r[:], ALU.mult, ALU.add),
              reads=[xt, r], writes=[r])
        if dbg == "A1":
            return
        x1 = x1_r.next()
        ln_tile(fw, nc, r, st_r.next(), rows, 1, 2, x1)
        fw.dma("sp", x1_d[T * 128:(T + 1) * 128, :], x1[:], reads=[x1], writes=[x1_d])
        if dbg == "A2":
            return
        h = h_r.next()
        mod_tile(fw, nc, x1, rows, 3, 4, h)
        hT32 = hT32_r.next()
        if dbg == "A3a":
            return
        if dbg == "A3b":
            transpose_tile(fw, nc, h, ident_f, pst.next(), [])
            return
        if dbg == "A3c":
            transpose_tile(fw, nc, h, ident_f, pst.next(), [("act", hT[:, :, T * 128:(T + 1) * 128], hT)])
            return
        if dbg == "A3d":
            transpose_tile(fw, nc, h, ident_f, pst.next(), [("dve", hT32[:], hT32)])
            return
        if dbg == "A3":
            transpose_tile(fw, nc, h, ident_f, pst.next(),
                           [("act", hT[:, :, T * 128:(T + 1) * 128], hT), ("dve", hT32[:], hT32)])
            return
        transpose_tile(fw, nc, h, ident_f, pst.next(),
                       [("act", hT[:, :, T * 128:(T + 1) * 128], hT), ("dve", hT32[:], hT32)])
        router_tile(fw, nc, hT32, wr_sb, brow, psl.next(), rt_r.next(), cw[:, T, :])
        cw_buf.w = rt_r.bufs[(rt_r.i - 1) % 2].w


def ffn_ln_phase(fw, nc, yacc, x1_d, rows, out_d):
    x_r = fw.ring(2, [128, D_MODEL], F32, "xc")
    r_r = fw.ring(2, [128, D_MODEL], F32, "rc")
    o_r = fw.ring(2, [128, D_MODEL], F32, "oc")
    st_r = fw.ring(2, [128, 16], F32, "stc")
    xt_next = x_r.next()
    fw.dma("sp", xt_next[:], x1_d[0:128, :], reads=[x1_d], writes=[xt_next])
    for T in range(NT):
        xt = xt_next
        if T + 1 < NT:
            xt_next = x_r.next()
            fw.dma("sp", xt_next[:], x1_d[(T + 1) * 128:(T + 2) * 128, :], reads=[x1_d], writes=[xt_next])
        r = r_r.next()
        fw.op("pool", lambda r=r, T=T: nc.gpsimd.tensor_mul(r[:], yacc[:, T, :], rows[:, 0, :]), reads=[yacc, rows], writes=[r])
        fw.op("dve", lambda r=r, xt=xt: nc.vector.scalar_tensor_tensor(r[:], xt[:], ALPHA, r[:], ALU.mult, ALU.add),
              reads=[xt, r], writes=[r])
        o = o_r.next()
        ln_tile(fw, nc, r, st_r.next(), rows, 1, 2, o)
        fw.dma("sp", out_d[T * 128:(T + 1) * 128, :], o[:], reads=[o], writes=[out_d])


TOK = 2048


def build_mid(stop=None):
    nc = new_nc()
    fw = FW(nc)
    hgT = fw.dram_in("hgT", [2048, TOK], BF16)
    x_d = fw.dram_in("x", [TOK, D_MODEL], F32)
    rows_d = fw.dram_in("rows", [12, D_MODEL], F32)
    w_out = fw.dram_in("w_out", [2048, D_MODEL], F32)
    wr_d = fw.dram_in("w_router", [D_MODEL, NE], F32)
    br_d = fw.dram_in("b_router", [1, NE], F32)
    wg_d = fw.dram_in("wg", [NE, D_MODEL, DFF], F32)
    wu_d = fw.dram_in("wu", [NE, D_MODEL, DFF], F32)
    wd_d = fw.dram_in("wd", [NE, DFF, D_MODEL], F32)
    wkv_d = fw.dram_in("w_kv", [D_MODEL, 2 * D_MODEL], F32)
    wq_d = fw.dram_in("wq1", [D_MODEL, D_MODEL], F32)
    cst_d = fw.dram_in("cst", [128, 128 + 8 * 16], F32)
    x2_o = fw.dram_out("x2", [TOK, D_MODEL], F32)
    KT_o = fw.dram_out("KT", [D_MODEL, TOK], BF16)
    V_o = fw.dram_out("V", [TOK, D_MODEL], BF16)
    km_o = fw.dram_out("kmT", [D_MODEL, 8], F32)
    QT_o = fw.dram_out("QT", [D_MODEL, TOK], BF16)
    kn_o = fw.dram_out("kn2", [16, 4], F32)
    x1_d = fw.dram_tmp("x1_tmp", [TOK, D_MODEL], F32)

    cst = fw.sbuf([128, 128 + 128], F32, "cst_sb")
    hsel_b = fw.sbuf([128, 8, 16], BF16, "hsel_b")
    wr_sb = fw.sbuf([128, 8, NE], F32, "wr_sb")
    brow = fw.sbuf([128, NE], F32, "brow")
    cw = fw.sbuf([128, NT, NE], F32, "cw")
    fw.dma("sp", cst[:], cst_d[:], writes=[cst])
    fw.dma("pool", hsel_b[:], cst_d[:, 128:256].rearrange("p (k h) -> p k h", k=8), writes=[hsel_b])
    fw.dma("sp", wr_sb[:], wr_d[:].rearrange("(k p) e -> p k e", p=128), writes=[wr_sb])
    fw.dma("sp", brow[:], br_d[0:1, :].to_broadcast([128, NE]), writes=[brow])

    m_h = fw.scope_begin()
    hT = fw.sbuf([128, 8, TOK], BF16, "hT")
    m_a = fw.scope_begin()
    wo_sb = fw.sbuf([128, 16, D_MODEL], BF16, "wo_sb")
    for k in range(16):
        fw.dma("pool", wo_sb[:, k, :], w_out[k * 128:(k + 1) * 128, :], writes=[wo_sb])
    rowsA = load_rows(fw, nc, rows_d, [0, 1, 2, 3, 4], "rowsA", plus1=(0, 3))
    hg_r = fw.ring(2, [128, 16, 128], BF16, "hg_t")
    hg_pending = {}

    def hg_load(T):
        hg = hg_r.next()
        fw.dma("sp", hg[:], hgT[:].rearrange("(k p) t -> p k t", p=128)[:, :, T * 128:(T + 1) * 128], writes=[hg])
        hg_pending[T] = hg

    hg_load(0)

    def y_fn(T, py):
        hg = hg_pending.pop(T)
        if T + 1 < NT:
            hg_load(T + 1)
        for half in range(2):
            fw.mm(py[:, half * 512:(half + 1) * 512],
                  [(hg[:, k, :], wo_sb[:, k, half * 512:(half + 1) * 512]) for k in range(16)], reads=[hg, wo_sb], writes=[py])

    mix_ln_phase(fw, nc, y_fn, x_d, x1_d, rowsA, hT, cw, cw, wr_sb, brow, cst,
                 dbg=(stop if stop in ("A0", "A1", "A2", "A3", "A4", "A3a", "A3b", "A3c", "A3d") else None))
    fw.scope_end(m_a)
    if stop is not None and stop.startswith("A"):
        fw.barrier([x1_d]); fw.close(); return nc
    yacc = fw.sbuf([128, NT, D_MODEL], F32, "yacc")
    moe_dense(fw, nc, hT, cw, cw, wg_d, wu_d, wd_d, yacc)
    if stop == "B":
        fw.barrier([x1_d]); fw.close(); return nc
    rowsC = load_rows(fw, nc, rows_d, [5, 6, 7], "rowsC", plus1=(5,))
    ffn_ln_phase(fw, nc, yacc, x1_d, rowsC, x2_o)
    fw.scope_end(m_h)
    if stop == "C":
        fw.barrier([x2_o]); fw.close(); return nc
    rowsD = load_rows(fw, nc, rows_d, [8, 9, 10, 11], "rowsD", plus1=(8, 10))
    wkv_sb = fw.sbuf([128, 8, 2 * D_MODEL], BF16, "wkv_sb")
    wq_sb = fw.sbuf([128, 8, D_MODEL], BF16, "wq_sb")
    for k in range(8):
        fw.dma("pool", wkv_sb[:, k, :], wkv_d[k * 128:(k + 1) * 128, :], writes=[wkv_sb])
        fw.dma("pool", wq_sb[:, k, :], wq_d[k * 128:(k + 1) * 128, :], writes=[wq_sb])
    x_r = fw.ring(2, [128, D_MODEL], F32, "xd")
    h_r = fw.ring(2, [128, D_MODEL], F32, "hd")
    hkT_r = fw.ring(2, [128, 8, 512], BF16, "hkT")
    hqT_r = fw.ring(2, [128, 8, 512], BF16, "hqT")
    kT_r = fw.ring(1, [128, 8, 512], BF16, "kTd")
    ksq_r = fw.ring(1, [128, 8, 512], BF16, "ksq")
    qT_r = fw.ring(1, [128, 8, 512], BF16, "qTd")
    v_r = fw.ring(2, [128, D_MODEL], BF16, "vd")
    km_sb = fw.sbuf([128, 8, 8], F32, "km_sb")
    kn_sb = fw.sbuf([16, 4], F32, "kn_sb")
    pst = fw.ring(1, [128, D_MODEL], F32, "pstd", psum=True)
    psm = fw.ring(4, [128, 512], F32, "psm", psum=True)
    for g in range(NT // 4):
        gs = slice(g * 512, (g + 1) * 512)
        hkT, hqT = hkT_r.next(), hqT_r.next()
        for tt in range(4):
            T = g * 4 + tt
            xt = x_r.next()
            fw.dma("sp", xt[:], x2_o[T * 128:(T + 1) * 128, :], reads=[x2_o], writes=[xt])
            for (isc, ish, dstT) in ((0, 1, hkT), (2, 3, hqT)):
                h = h_r.next()
                mod_tile(fw, nc, xt, rowsD, isc, ish, h)
                transpose_tile(fw, nc, h, cst, pst.next(), [("act", dstT[:, :, tt * 128:(tt + 1) * 128], dstT)])
        kTg, qTg, ksq = kT_r.next(), qT_r.next(), ksq_r.next()
        for fc in range(8):
            p = psm.next()
            fw.mm(p[:], [(wkv_sb[:, k, fc * 128:(fc + 1) * 128], hkT[:, k, :]) for k in range(8)], reads=[wkv_sb, hkT], writes=[p])
            fw.op("act", lambda p=p, fc=fc: nc.scalar.copy(kTg[:, fc, :], p[:]), reads=[p], writes=[kTg])
            fw.op("act", lambda p=p, fc=fc: nc.scalar.activation(ksq[:, fc, :], p[:], AF.Square), reads=[p], writes=[ksq])
            fw.op("dve", lambda p=p, fc=fc, g=g: nc.vector.tensor_reduce(
                km_sb[:, fc, 2 * g:2 * g + 2], p[:].rearrange("p (b t) -> p b t", b=2), AX.X, ALU.add), reads=[p], writes=[km_sb])
        fw.dma("sp", KT_o[:].rearrange("(c p) t -> p c t", p=128)[:, :, gs], kTg[:], reads=[kTg], writes=[KT_o])
        p = psm.next()
        fw.mm(p[0:16, :], [(hsel_b[:, fc, :], ksq[:, fc, :]) for fc in range(8)], reads=[hsel_b, ksq], writes=[p])
        fw.op("dve", lambda p=p, g=g: nc.vector.reduce_max(kn_sb[:, g:g + 1], p[0:16, :], AX.X), reads=[p], writes=[kn_sb])
        for fc in range(8):
            p = psm.next()
            fw.mm(p[:], [(wq_sb[:, k, fc * 128:(fc + 1) * 128], hqT[:, k, :]) for k in range(8)], reads=[wq_sb, hqT], writes=[p])
            fw.op("act", lambda p=p, fc=fc: nc.scalar.mul(qTg[:, fc, :], p[:], 0.125), reads=[p], writes=[qTg])
        fw.dma("sp", QT_o[:].rearrange("(c p) t -> p c t", p=128)[:, :, gs], qTg[:], reads=[qTg], writes=[QT_o])
        for tt in range(4):
            T = g * 4 + tt
            vt = v_r.next()
            for half in range(2):
                p = psm.next()
                fw.mm(p[:], [(hkT[:, k, tt * 128:(tt + 1) * 128], wkv_sb[:, k, D_MODEL + half * 512:D_MODEL + (half + 1) * 512])
                             for k in range(8)], reads=[wkv_sb, hkT], writes=[p])
                fw.op("dve", lambda p=p, half=half, vt=vt: nc.vector.tensor_copy(vt[:, half * 512:(half + 1) * 512], p[:]),
                      reads=[p], writes=[vt])
            fw.dma("act", V_o[T * 128:(T + 1) * 128, :], vt[:], reads=[vt], writes=[V_o])
    fw.op("dve", lambda: nc.vector.tensor_scalar(km_sb[:], km_sb[:], 1.0 / 256.0, None, ALU.mult), reads=[km_sb], writes=[km_sb])
    fw.dma("sp", km_o[:].rearrange("(c p) b -> p c b", p=128), km_sb[:], reads=[km_sb], writes=[km_o])
    fw.dma("sp", kn_o[:], kn_sb[:], reads=[kn_sb], writes=[kn_o])
    for e in ("sp", "act"):
        fw.wait_all(e, [x2_o, KT_o, V_o, km_o, QT_o, kn_o])
    fw.close()
    return nc


def mid_consts():
    cst = np.zeros((128, 256), np.float32)
    cst[:, :128] = np.eye(128, dtype=np.float32)
    hs = np.zeros((128, 8, 16), np.float32)
    for k in range(8):
        for p in range(128):
            hs[p, k, (k * 128 + p) // 64] = 1.0
    cst[:, 128:] = hs.reshape(128, 128)
    return cst


def cond_rows(inp, cond, b):
    g = lambda l, s, w: cond[b, mod_off(l, s, w):mod_off(l, s, w) + D_MODEL]
    kvo = 2 * 6 * D_MODEL
    return [g(0, 0, 2), inp["ln_g"][0, 0], inp["ln_b"][0, 0], g(0, 1, 1), g(0, 1, 0),
            g(0, 1, 2), inp["ln_g"][0, 1], inp["ln_b"][0, 1],
            cond[b, kvo + D_MODEL:kvo + 2 * D_MODEL], cond[b, kvo:kvo + D_MODEL],
            g(1, 0, 1), g(1, 0, 0)]


def run_mid(inp, cond, core_res):
    maps = []
    cst = mid_consts()
    for c in range(NCORES):
        b, seg = divmod(c, 4)
        ts = slice(seg * TOK, (seg + 1) * TOK)
        hgT = np.ascontiguousarray(np.concatenate([core_res[b * 4 + hd]["hgT"][:, ts] for hd in range(4)], axis=0))
        maps.append({"hgT": hgT, "x": np.ascontiguousarray(inp["x"][b, ts]),
                     "rows": np.ascontiguousarray(np.stack(cond_rows(inp, cond, b)).astype(np.float32)),
                     "w_out": inp["a_w_out"][0], "w_router": inp["moe_w_router"], "b_router": inp["moe_b_router"][None, :],
                     "wg": inp["moe_w_gate"][0], "wu": inp["moe_w_up"][0], "wd": inp["moe_w_down"][0],
                     "w_kv": inp["b_w_kv"], "wq1": inp["b_wq"][0], "cst": cst})
    return run(build_mid(), maps)


NBLK = 32
NAUX = 33


def build_select():
    nc = new_nc()
    fw = FW(nc)
    QT = fw.dram_in("QT", [D_MODEL, TOK], BF16)
    km_d = fw.dram_in("kmT", [D_MODEL, NBLK], F32)
    kn_d = fw.dram_in("kn2", [1, 16 * 16], F32)
    tab_d = fw.dram_in("tab", [128, NT, 2 * NBLK + 16], F32)
    cst_d = fw.dram_in("cst", [128, 256], F32)
    aux_o = fw.dram_out("auxR", [16, NAUX, TOK], BF16)
    QT_sb = fw.sbuf([128, 8, TOK], BF16, "QT_sb")
    Qsq = fw.sbuf([128, 8, TOK], BF16, "Qsq")
    km_b = fw.sbuf([128, 8, NBLK], BF16, "km_b")
    tab = fw.sbuf([128, NT, 2 * NBLK + 16], F32, "tab")
    cst = fw.sbuf([128, 256], F32, "cst_sb")
    hsel_b = fw.sbuf([128, 8, 16], BF16, "hsel_b")
    knb = fw.sbuf([128, 16, 16], F32, "knb")
    knmax = fw.sbuf([128, 16], F32, "knmax")
    fw.dma("sp", QT_sb[:], QT[:].rearrange("(c p) t -> p c t", p=128), writes=[QT_sb])
    fw.dma("pool", km_b[:], km_d[:].rearrange("(c p) n -> p c n", p=128), writes=[km_b])
    fw.dma("sp", tab[:], tab_d[:], writes=[tab])
    fw.dma("sp", cst[:], cst_d[:], writes=[cst])
    fw.dma("pool", hsel_b[:], cst_d[:, 128:256].rearrange("p (k h) -> p k h", k=8), writes=[hsel_b])
    fw.dma("sp", knb[:].rearrange("p h v -> p (h v)"), kn_d[0:1, :].to_broadcast([128, 256]), writes=[knb])
    fw.op("dve", lambda: nc.vector.tensor_reduce(knmax[:], knb[:], AX.X, ALU.max), reads=[knb], writes=[knmax])
    for c in range(8):
        fw.op("pool", lambda c=c: nc.gpsimd.tensor_mul(Qsq[:, c, :], QT_sb[:, c, :], QT_sb[:, c, :]), reads=[QT_sb], writes=[Qsq])
    pg_r = fw.ring(2, [128, 512], F32, "pg", psum=True)
    pn_r = fw.ring(1, [128, 512], F32, "pn", psum=True)
    ptr_r = fw.ring(4, [128, 512], F32, "ptr", psum=True)
    gm_r = fw.ring(2, [128, 16, NBLK], F32, "gm")
    t8_r = fw.ring(2, [128, 16, 8], F32, "t8")
    mt_r = fw.ring(2, [128, 16, NAUX], F32, "mt")
    mm_r = fw.ring(2, [128, 16], F32, "mq")
    ax_r = fw.ring(2, [NAUX, 16, 128], BF16, "ax")
    V = nc.vector
    for T in range(NT):
        qs = slice(T * 128, (T + 1) * 128)
        pg = pg_r.next()
        for h in range(16):
            pb = (h % 2) * 64
            fw.mm(pg[:, h * NBLK:(h + 1) * NBLK], [(QT_sb[pb:pb + 64, h // 2, qs], km_b[pb:pb + 64, h // 2, :])],
                  reads=[QT_sb, km_b], writes=[pg])
        gm, t8, mt, mq = gm_r.next(), t8_r.next(), mt_r.next(), mm_r.next()
        pastb = tab[:, T, 0:NBLK].unsqueeze(1).to_broadcast([128, 16, NBLK])
        notown = tab[:, T, NBLK:2 * NBLK].unsqueeze(1).to_broadcast([128, 16, NBLK])
        fw.op("dve", lambda: V.tensor_tensor(gm[:], pg[:].rearrange("p (h n) -> p h n", h=16), pastb, ALU.add),
              reads=[pg, tab], writes=[gm])
        for h in range(16):
            fw.op("dve", lambda h=h: V.max(t8[:, h, :], gm[:, h, :]), reads=[gm], writes=[t8])
        fw.op("dve", lambda: V.tensor_tensor(gm[:], gm[:], t8[:, :, 2:3].to_broadcast([128, 16, NBLK]), ALU.is_ge),
              reads=[gm, t8], writes=[gm])
        fw.op("dve", lambda: V.tensor_scalar(gm[:], gm[:], -1.0, BIG, ALU.add, ALU.mult), reads=[gm], writes=[gm])
        fw.op("dve", lambda: V.tensor_tensor(gm[:], gm[:], pastb, ALU.min), reads=[gm, tab], writes=[gm])
        fw.op("dve", lambda: V.tensor_tensor(mt[:, :, 0:NBLK], gm[:], notown, ALU.mult), reads=[gm, tab], writes=[mt])
        pn = pn_r.next()
        fw.mm(pn[:, 0:16], [(Qsq[:, c, qs], hsel_b[:, c, :]) for c in range(8)], reads=[Qsq, hsel_b], writes=[pn])
        fw.op("dve", lambda: V.tensor_tensor(mq[:], pn[:, 0:16], knmax[:], ALU.mult), reads=[pn, knmax], writes=[mq])
        fw.op("act", lambda: nc.scalar.activation(mq[:], mq[:], AF.Sqrt), reads=[mq], writes=[mq])
        fw.op("dve", lambda: V.scalar_tensor_tensor(mt[:, :, NBLK], mq[:], -1.0, tab[:, T, 2 * NBLK:2 * NBLK + 16],
                                                    ALU.mult, ALU.subtract), reads=[mq, tab], writes=[mt])
        ax = ax_r.next()
        for hq in range(4):
            pt = ptr_r.next()
            for i in range(4):
                h = hq * 4 + i
                fw.op("pe", lambda h=h, i=i, pt=pt: nc.tensor.transpose(pt[0:NAUX, i * 128:(i + 1) * 128], mt[:, h, :], cst[:, 0:128]),
                      reads=[mt, cst], writes=[pt], inc=(i == 3))
            fw.op("act", lambda pt=pt, hq=hq: nc.scalar.copy(ax[:, hq * 4:(hq + 1) * 4, :],
                                                             pt[0:NAUX, :].rearrange("p (h t) -> p h t", h=4)),
                  reads=[pt], writes=[ax])
        fw.dma("sp", aux_o[:].rearrange("h r t -> r h t")[:, :, qs], ax[:], reads=[ax], writes=[aux_o])
    fw.wait_all("sp", [aux_o])
    fw.close()
    return nc


def alibi_slopes():
    return np.exp2(-8.0 * (np.arange(16, dtype=np.float32) + 1.0) / 16).astype(np.float32)


def run_select(mid):
    maps = []
    cst = mid_consts()
    sl = alibi_slopes()
    for c in range(NCORES):
        b, seg = divmod(c, 4)
        kmT = np.ascontiguousarray(np.concatenate([mid[b * 4 + s]["kmT"] for s in range(4)], axis=1))
        kn2 = np.ascontiguousarray(np.concatenate([mid[b * 4 + s]["kn2"] for s in range(4)], axis=1).reshape(1, 256))
        tab = np.zeros((128, NT, 2 * NBLK + 16), np.float32)
        for T in range(NT):
            own = (seg * TOK + T * 128) // 256
            tab[:, T, 0:NBLK] = np.where(np.arange(NBLK) < own, 0.0, -BIG)[None, :]
            tab[:, T, NBLK:2 * NBLK] = np.where(np.arange(NBLK) == own, 0.0, 1.0)[None, :]
            iq = (T % 4) * 128 + np.arange(128)
            tab[:, T, 2 * NBLK:] = sl[None, :] * iq[:, None]
        maps.append({"QT": mid[c]["QT"], "kmT": kmT, "kn2": kn2, "tab": tab, "cst": cst})
    return run(build_select(), maps)


NQG = SEQ // 512
NKA = 16


def build_attn(nheads=16):
    nc = new_nc()
    fw = FW(nc)
    KTr = fw.dram_in("KTr", [D_MODEL, NKA * 128], BF16)
    Vr = fw.dram_in("Vr", [16, 128, NKA * 64], BF16)
    QT = fw.dram_in("QT", [D_MODEL, SEQ], BF16)
    aux = fw.dram_in("auxR", [16, NAUX, SEQ], BF16)
    auxK = fw.dram_in("auxK", [NAUX, NKA * 128], BF16)
    bias_d = fw.dram_in("biasT", [128, 16 * NKA], F32)
    cst_d = fw.dram_in("cst", [128, 128 + 512], F32)
    Op = fw.dram_out("Opart", [16, 65, SEQ], F32)
    bias = fw.sbuf([128, 16 * NKA], F32, "bias")
    cb = fw.sbuf([128, 128 + 512], BF16, "cb")
    fw.dma("sp", bias[:], bias_d[:], writes=[bias])
    fw.dma("pool", cb[:], cst_d[:], writes=[cb])
    KL_r = fw.ring(2, [128, NKA * 128], BF16, "KL")
    Vx_r = fw.ring(2, [128, NKA, 65], BF16, "Vx")
    QR_r = fw.ring(3, [128, 512], BF16, "QR")
    PT_r = fw.ring(4, [128, 512], BF16, "PT")
    os_r = fw.ring(2, [128, 512], F32, "osb")
    psS = fw.ring(3, [128, 512], F32, "psS", psum=True)
    psO = fw.ring(2, [128, 512], F32, "psO", psum=True)
    for KL in KL_r.bufs:
        fw.dma("sp", KL[64:64 + NAUX, :], auxK[:], writes=[KL])
    for Vx in Vx_r.bufs:
        fw.op("pool", lambda Vx=Vx: nc.gpsimd.memset(Vx[:], 1.0), writes=[Vx])
    def load_head(h):
        KL, Vx = KL_r.next(), Vx_r.next()
        fw.dma("sp", KL[0:64, :], KTr[h * 64:(h + 1) * 64, :], writes=[KL])
        fw.dma("sp", Vx[:, :, 0:64], Vr[h].rearrange("p (a d) -> p a d", a=NKA), writes=[Vx])
        return KL, Vx

    def load_q(h, j):
        qs = slice(j * 512, (j + 1) * 512)
        QR = QR_r.next()
        fw.dma("sp", QR[0:64, :], QT[h * 64:(h + 1) * 64, qs], writes=[QR])
        fw.dma("sp", QR[64:64 + NAUX, :], aux[h, :, qs], writes=[QR])
        return QR

    steps = [(h, j) for h in range(nheads) for j in range(NQG)]
    head_next = load_head(0)
    q_next = load_q(0, 0)
    for si, (h, j) in enumerate(steps):
        if j == 0:
            KL, Vx = head_next
            if h + 1 < nheads:
                head_next = load_head(h + 1)
        QR = q_next
        if si + 1 < len(steps):
            q_next = load_q(*steps[si + 1])
        qs = slice(j * 512, (j + 1) * 512)
        po = psO.next()

        def qk(a):
            ps = psS.next()
            pairs = [(KL[0:64 + NAUX, a * 128:(a + 1) * 128], QR[0:64 + NAUX, :])]
            if a == j:
                pairs.append((cb[:, 0:128], cb[:, 128:640]))
            fw.mm(ps[:], pairs, reads=[KL, QR, cb], writes=[ps])
            return ps

        ps_cur = qk(0)
        for a in range(j + 1):
            ps = ps_cur
            if a < j:
                ps_cur = qk(a + 1)
            PT = PT_r.next()
            fw.op("act", lambda ps=ps, PT=PT, h=h, j=j, a=a: nc.scalar.activation(
                PT[:], ps[:], AF.Exp, bias=bias[:, h * NKA + (j - a):h * NKA + (j - a) + 1]),
                reads=[ps, bias], writes=[PT])
            fw.op("pe", lambda po=po, Vx=Vx, PT=PT, a=a, j=j: nc.tensor.matmul(
                po[0:65, :], Vx[:, a, :], PT[:], start=(a == 0), stop=(a == j)),
                reads=[Vx, PT], writes=[po], inc=True)
        osb = os_r.next()
        fw.op("dve", lambda osb=osb, po=po: nc.vector.tensor_copy(osb[0:65, :], po[0:65, :]), reads=[po], writes=[osb])
        fw.dma("sp", Op[h, :, qs], osb[0:65, :], reads=[osb], writes=[Op])
    fw.wait_all("sp", [Op])
    fw.close()
    return nc


def run_attn(mid, sel, nheads=16):
    maps = []
    sl = alibi_slopes()
    for c in range(NCORES):
        b, r = divmod(c, 4)
        KT = np.concatenate([mid[b * 4 + s]["KT"] for s in range(4)], axis=1)
        V = np.concatenate([mid[b * 4 + s]["V"] for s in range(4)], axis=0)
        QT = np.ascontiguousarray(np.concatenate([mid[b * 4 + s]["QT"] for s in range(4)], axis=1))
        aux = np.ascontiguousarray(np.concatenate([sel[b * 4 + s]["auxR"] for s in range(4)], axis=2))
        kts = [4 * a + r for a in range(NKA)]
        KTr = np.ascontiguousarray(np.concatenate([KT[:, kt * 128:(kt + 1) * 128] for kt in kts], axis=1))
        Vsel = np.stack([V[kt * 128:(kt + 1) * 128, :] for kt in kts], axis=0)
        Vr = np.ascontiguousarray(Vsel.reshape(NKA, 128, 16, 64).transpose(2, 1, 0, 3).reshape(16, 128, NKA * 64))
        auxK = np.zeros((NAUX, NKA * 128), np.float32)
        for a, kt in enumerate(kts):
            auxK[kt // 2, a * 128:(a + 1) * 128] = 1.0
        auxK[NBLK, :] = 1.0
        ik = np.arange(128, dtype=np.float32)
        biasT = np.zeros((128, 16, NKA), np.float32)
        for dj in range(NKA):
            biasT[:, :, dj] = sl[None, :] * (ik[:, None] + 128.0 * r - 512.0 * dj)
        cst = np.zeros((128, 128 + 512), np.float32)
        cst[:, :128] = np.eye(128)
        kk, qq = np.meshgrid(np.arange(128) + 128 * r, np.arange(512), indexing="ij")
        same_blk = (kk // 256) == (qq // 256)
        cst[:, 128:] = np.where(same_blk & (kk > qq), -BIG, 0.0)
        maps.append({"KTr": KTr, "Vr": Vr, "QT": QT, "auxR": aux, "auxK": auxK.astype(NPBF),
                     "biasT": np.ascontiguousarray(biasT.reshape(128, 16 * NKA)), "cst": cst})
    return run(build_attn(nheads), maps)


def build_tail():
    nc = new_nc()
    fw = FW(nc)
    Op = fw.dram_in("Op4", [4, 16, 65, TOK], F32)
    x_d = fw.dram_in("x2", [TOK, D_MODEL], F32)
    rows_d = fw.dram_in("rows", [8, D_MODEL], F32)
    wo_d = fw.dram_in("wo", [D_MODEL, D_MODEL], F32)
    wr_d = fw.dram_in("w_router", [D_MODEL, NE], F32)
    br_d = fw.dram_in("b_router", [1, NE], F32)
    wg_d = fw.dram_in("wg", [NE, D_MODEL, DFF], F32)
    wu_d = fw.dram_in("wu", [NE, D_MODEL, DFF], F32)
    wd_d = fw.dram_in("wd", [NE, DFF, D_MODEL], F32)
    cst_d = fw.dram_in("cst", [128, 256], F32)
    out_o = fw.dram_out("out", [TOK, D_MODEL], F32)
    x3_d = fw.dram_tmp("x3_tmp", [TOK, D_MODEL], F32)

    cst = fw.sbuf([128, 256], F32, "cst_sb")
    wr_sb = fw.sbuf([128, 8, NE], F32, "wr_sb")
    brow = fw.sbuf([128, NE], F32, "brow")
    cw = fw.sbuf([128, NT, NE], F32, "cw")
    fw.dma("sp", cst[:], cst_d[:], writes=[cst])
    fw.dma("sp", wr_sb[:], wr_d[:].rearrange("(k p) e -> p k e", p=128), writes=[wr_sb])
    fw.dma("sp", brow[:], br_d[0:1, :].to_broadcast([128, NE]), writes=[brow])
    O = fw.sbuf([128, NT, D_MODEL], F32, "O_tm")
    m_h = fw.scope_begin()
    hT = fw.sbuf([128, 8, TOK], BF16, "hT")
    m_a = fw.scope_begin()
    m_o = fw.scope_begin()
    op_r = fw.ring(2, [65, 4, TOK // 2], F32, "op4")
    os_r = fw.ring(2, [65, TOK // 2], F32, "osum")
    rl_r = fw.ring(2, [128, 4], F32, "rl")
    ptr = fw.ring(2, [128, 4, 128], F32, "ptr6", psum=True)
    for h in range(16):
        for hf in range(2):
            cs = slice(hf * (TOK // 2), (hf + 1) * (TOK // 2))
            o4, osum = op_r.next(), os_r.next()
            for r in range(4):
                fw.dma("sp" if r % 2 == 0 else "act", o4[:, r, :], Op[r, h, :, cs], writes=[o4])
            fw.op("dve", lambda o4=o4, osum=osum: nc.vector.tensor_add(osum[:], o4[:, 0, :], o4[:, 1, :]), reads=[o4], writes=[osum])
            fw.op("pool", lambda o4=o4: nc.gpsimd.tensor_add(o4[:, 2, :], o4[:, 2, :], o4[:, 3, :]), reads=[o4], writes=[o4])
            fw.op("dve", lambda o4=o4, osum=osum: nc.vector.tensor_add(osum[:], osum[:], o4[:, 2, :]), reads=[o4, osum], writes=[osum])
            for tq in range(2):
                pt, rl = ptr.next(), rl_r.next()
                for i in range(4):
                    ti = tq * 4 + i
                    fw.op("pe", lambda pt=pt, i=i, ti=ti, osum=osum: nc.tensor.transpose(
                        pt[:, i, 0:65], osum[:, ti * 128:(ti + 1) * 128], cst[0:65, 0:65]),
                        reads=[osum, cst], writes=[pt], inc=(i == 3))
                fw.op("dve", lambda pt=pt, rl=rl: nc.vector.reciprocal(rl[:], pt[:, :, 64]), reads=[pt], writes=[rl])
                T0 = hf * 8 + tq * 4
                fw.op("dve", lambda pt=pt, rl=rl, T0=T0, h=h: nc.vector.tensor_tensor(
                    O[:, T0:T0 + 4, h * 64:(h + 1) * 64], pt[:, :, 0:64], rl[:].unsqueeze(2).to_broadcast([128, 4, 64]), ALU.mult),
                    reads=[pt, rl], writes=[O])
    fw.scope_end(m_o)
    wo_sb = fw.sbuf([128, 8, D_MODEL], BF16, "wo_sb")
    for k in range(8):
        fw.dma("pool", wo_sb[:, k, :], wo_d[k * 128:(k + 1) * 128, :], writes=[wo_sb])
    rowsA = load_rows(fw, nc, rows_d, [0, 1, 2, 3, 4], "rowsA", plus1=(0, 3))
    OT_r = fw.ring(2, [128, 8, 128], BF16, "OT")
    pst = fw.ring(1, [128, D_MODEL], F32, "pst", psum=True)

    def y_fn(T, py):
        OT = OT_r.next()
        for k in range(8):
            fw.op("pe", lambda k=k: nc.tensor.transpose(pst.bufs[0][:, k * 128:(k + 1) * 128], O[:, T, k * 128:(k + 1) * 128],
                                                        cst[:, 0:128]), reads=[O, cst], writes=[pst.bufs[0]], inc=(k == 7))
        fw.op("act", lambda: nc.scalar.copy(OT[:], pst.bufs[0][:].rearrange("p (k t) -> p k t", k=8)),
              reads=[pst.bufs[0]], writes=[OT])
        for half in range(2):
            fw.mm(py[:, half * 512:(half + 1) * 512],
                  [(OT[:, k, :], wo_sb[:, k, half * 512:(half + 1) * 512]) for k in range(8)], reads=[OT, wo_sb], writes=[py])

    mix_ln_phase(fw, nc, y_fn, x_d, x3_d, rowsA, hT, cw, cw, wr_sb, brow, cst, pst=pst)
    fw.scope_end(m_a)
    yacc = O
    moe_dense(fw, nc, hT, cw, cw, wg_d, wu_d, wd_d, yacc)
    rowsC = load_rows(fw, nc, rows_d, [5, 6, 7], "rowsC", plus1=(5,))
    ffn_ln_phase(fw, nc, yacc, x3_d, rowsC, out_o)
    fw.scope_end(m_h)
    fw.wait_all("sp", [out_o])
    fw.close()
    return nc


def run_tail(inp, cond, mid, att):
    maps = []
    cst = mid_consts()
    for c in range(NCORES):
        b, seg = divmod(c, 4)
        ts = slice(seg * TOK, (seg + 1) * TOK)
        Op4 = np.ascontiguousarray(np.stack([att[b * 4 + r]["Opart"][:, :, ts] for r in range(4)]))
        g = lambda l, s, w: cond[b, mod_off(l, s, w):mod_off(l, s, w) + D_MODEL]
        rows = np.stack([g(1, 0, 2), inp["ln_g"][1, 0], inp["ln_b"][1, 0], g(1, 1, 1), g(1, 1, 0),
                         g(1, 1, 2), inp["ln_g"][1, 1], inp["ln_b"][1, 1]]).astype(np.float32)
        maps.append({"Op4": Op4, "x2": mid[c]["x2"], "rows": np.ascontiguousarray(rows), "wo": inp["b_wo"][0],
                     "w_router": inp["moe_w_router"], "b_router": inp["moe_b_router"][None, :],
                     "wg": inp["moe_w_gate"][1], "wu": inp["moe_w_up"][1], "wd": inp["moe_w_down"][1], "cst": cst})
    return run(build_tail(), maps)


def kernel(**inp):
    inp = {k: np.asarray(v) for k, v in inp.items()}
    cond = run_cond(inp)
    front = run_mlstm_front(inp, cond)
    core = run_mlstm_core(inp, front)
    del front
    mid = run_mid(inp, cond, core)
    del core
    sel = run_select(mid)
    att = run_attn(mid, sel)
    res = run_tail(inp, cond, mid, att)
    out = np.empty((2, SEQ, D_MODEL), np.float32)
    for c in range(NCORES):
        b, seg = divmod(c, 4)
        out[b, seg * TOK:(seg + 1) * TOK] = res[c]["out"]
    return out
```

```python
import numpy as np
import ml_dtypes
import concourse.bass as bass
import concourse.mybir as mybir
from concourse.bass_utils import run_bass_kernel_spmd

F32 = mybir.dt.float32
BF16 = mybir.dt.bfloat16
AF = mybir.ActivationFunctionType
ALU = mybir.AluOpType
AX = mybir.AxisListType
NPBF = ml_dtypes.bfloat16

D_MODEL = 1024
SEQ = 8192
NCORES = 8
LN_EPS = 1e-5
ALPHA = 4.0 ** 0.25
BIG = 30000.0


class Buf:
    __slots__ = ("t", "name", "w", "r", "dma_sem", "dma_cnt", "psum")

    def __init__(self, t, name):
        self.t = t
        self.name = name
        self.psum = False
        self.w = None
        self.r = []
        self.dma_sem = None
        self.dma_cnt = 0

    def __getitem__(self, k):
        return self.t[k]


class Ring:
    def __init__(self, bufs):
        self.bufs = bufs
        self.i = 0

    def next(self):
        b = self.bufs[self.i % len(self.bufs)]
        self.i += 1
        return b


class FW:
    def __init__(self, nc):
        self.nc = nc
        self.stack = []
        self.sem_stack = []
        self.eng = {"pe": nc.tensor, "act": nc.scalar, "dve": nc.vector, "pool": nc.gpsimd, "sp": nc.sync}
        self.sem = {}
        self.cnt = {}
        for k in self.eng:
            self.sem[k] = self._enter_sem(nc.semaphore("s_" + k))
            self.cnt[k] = 0
        self.known = {e: {} for e in self.eng}
        self.nbuf = 0
        self.n_inst = 0
        self.live = []

    def _enter(self, cm):
        v = cm.__enter__()
        self.stack.append(cm)
        return v

    def _enter_sem(self, cm):
        v = cm.__enter__()
        self.sem_stack.append(cm)
        return v

    def close(self):
        while self.stack:
            self.stack.pop().__exit__(None, None, None)
        while self.sem_stack:
            self.sem_stack.pop().__exit__(None, None, None)

    def sbuf(self, shape, dt, name=None):
        self.nbuf += 1
        name = (name or f"sb{self.nbuf}") + "_s"
        b = Buf(self._enter(self.nc.sbuf_tensor(name, list(shape), dt)), name)
        self.live.append((len(self.stack), b))
        return b

    def psum(self, shape, dt, name=None):
        self.nbuf += 1
        name = (name or f"ps{self.nbuf}") + "_p"
        b = Buf(self._enter(self.nc.psum_tensor(name, list(shape), dt)), name)
        b.psum = True
        self.live.append((len(self.stack), b))
        return b

    def ring(self, n, shape, dt, name, psum=False):
        mk = self.psum if psum else self.sbuf
        return Ring([mk(shape, dt, f"{name}{i}") for i in range(n)])

    def dram_in(self, name, shape, dt):
        return Buf(self.nc.dram_tensor(name, list(shape), dt, kind="ExternalInput").ap(), name)

    def dram_out(self, name, shape, dt):
        return Buf(self.nc.dram_tensor(name, list(shape), dt, kind="ExternalOutput").ap(), name)

    def dram_tmp(self, name, shape, dt):
        return Buf(self.nc.dram_tensor(name, list(shape), dt, kind="Internal").ap(), name)

    def _need(self, e, dep):
        if dep is None:
            return
        key, val = dep
        if key == e and val > self.cnt[e]:
            return
        if self.known[e].get(key, 0) >= val:
            return
        sem = key[1] if isinstance(key, tuple) else self.sem[key]
        self.eng[e].wait_ge(sem, val)
        self.known[e][key] = val

    def _deps(self, e, reads, writes):
        for b in reads:
            self._need(e, b.w)
        for b in writes:
            self._need(e, b.w)
            for r in b.r:
                self._need(e, r)

    def _mark(self, tag, reads, writes):
        for b in reads:
            b.r.append(tag)
            if len(b.r) > 64:
                last = {}
                for k, v in b.r:
                    last[k] = max(v, last.get(k, 0))
                b.r = list(last.items())
        for b in writes:
            b.w = tag
            b.r = []

    def op(self, e, fn, reads=(), writes=(), inc=True):
        pr = [b for b in reads if b.psum]
        if pr:
            reads = [b for b in reads if not b.psum]
            writes = list(writes) + [b for b in pr if b not in writes]
        self._deps(e, reads, writes)
        ins = fn()
        self.n_inst += 1
        if inc:
            self.cnt[e] += 1
            ins.then_inc(self.sem[e], 1)
            tag = (e, self.cnt[e])
        else:
            tag = (e, self.cnt[e] + 1)
        self._mark(tag, reads, writes)
        return ins

    def dma(self, e, out_ap, in_ap, reads=(), writes=(), sembuf=None, **kw):
        self._deps(e, reads, writes)
        sb = sembuf or (writes[0] if writes else reads[0])
        if sb.dma_sem is None:
            self.nbuf += 1
            sb.dma_sem = self._enter_sem(self.nc.semaphore(f"d{self.nbuf}"))
        ins = self.eng[e].dma_start(out=out_ap, in_=in_ap, **kw)
        sb.dma_cnt += 16
        ins.then_inc(sb.dma_sem, 16)
        self.n_inst += 1
        tag = (("dma", sb.dma_sem, id(sb)), sb.dma_cnt)
        self._mark(tag, reads, writes)
        return ins

    def scope_begin(self):
        return len(self.stack)

    def scope_end(self, mark):
        dead = [b for d, b in self.live if d > mark]
        self.live = [(d, b) for d, b in self.live if d <= mark]
        self.barrier(dead)
        while len(self.stack) > mark:
            self.stack.pop().__exit__(None, None, None)

    def barrier(self, bufs=()):
        for e in ("pe", "act", "dve", "pool", "sp"):
            for f in ("pe", "act", "dve", "pool"):
                if f != e and self.cnt[f] > 0:
                    self._need(e, (f, self.cnt[f]))
            self.wait_all(e, bufs)

    def wait_all(self, e, bufs):
        for b in bufs:
            self._need(e, b.w)
            for r in b.r:
                self._need(e, r)

    def mm(self, out_ap, pairs, reads, writes, start=True, stop=True):
        n = len(pairs)
        for i, (l, r) in enumerate(pairs):
            self.op("pe", lambda l=l, r=r, i=i: self.nc.tensor.matmul(
                out_ap, l, r, start=(start and i == 0), stop=(stop and i == n - 1)),
                reads=reads, writes=writes, inc=(i == n - 1))


def new_nc():
    return bass.Bass("TRN2", target_bir_lowering=False)


def run(nc, in_maps):
    res = run_bass_kernel_spmd(nc, in_maps, core_ids=list(range(NCORES)))
    et = getattr(res, "exec_time_ns", None)
    if et:
        print(f"[kernel] launch exec_time_ns={et}", flush=True)
    return res.results


NCOND = 14336
CW = NCOND // NCORES


def build_cond():
    nc = new_nc()
    fw = FW(nc)
    cT = fw.dram_in("cT", [128, 8, 2], F32)
    w = fw.dram_in("w", [D_MODEL, CW], F32)
    b = fw.dram_in("b", [1, CW], F32)
    o = fw.dram_out("cond", [2, CW], F32)
    c_sb = fw.sbuf([128, 8, 2], F32, "c_sb")
    cs = fw.sbuf([128, 8, 2], F32, "cs")
    w_sb = fw.sbuf([128, 8, CW], F32, "w_sb")
    b_sb = fw.sbuf([2, CW], F32, "b_sb")
    o_sb = fw.sbuf([2, CW], F32, "o_sb")
    ps = fw.ring(4, [2, 448], F32, "ps", psum=True)
    fw.dma("sp", c_sb[:], cT[:], writes=[c_sb])
    for ch in range(8):
        fw.dma("sp" if ch % 2 == 0 else "act", w_sb[:, ch, :], w[ch * 128:(ch + 1) * 128, :], writes=[w_sb])
    for r in range(2):
        fw.dma("sp", b_sb[r:r + 1, :], b[:], writes=[b_sb])
    fw.op("act", lambda: nc.scalar.activation(cs[:], c_sb[:], AF.Silu), reads=[c_sb], writes=[cs])
    for j in range(4):
        p = ps.next()
        fw.mm(p[:], [(cs[:, ch, :], w_sb[:, ch, j * 448:(j + 1) * 448]) for ch in range(8)],
              reads=[cs, w_sb], writes=[p])
        fw.op("dve", lambda p=p, j=j: nc.vector.tensor_tensor(
            o_sb[:, j * 448:(j + 1) * 448], p[:], b_sb[:, j * 448:(j + 1) * 448], ALU.add),
            reads=[p, b_sb], writes=[o_sb])
    fw.dma("sp", o[:], o_sb[:], reads=[o_sb], writes=[o])
    fw.wait_all("sp", [o])
    fw.close()
    return nc


def run_cond(inp):
    c = inp["c"]
    cT = np.ascontiguousarray(c.reshape(2, 8, 128).transpose(2, 1, 0))
    w_ada, b_ada = inp["w_ada"], inp["b_ada"]
    maps = []
    for i in range(NCORES):
        maps.append({"cT": cT, "w": np.ascontiguousarray(w_ada[:, i * CW:(i + 1) * CW]),
                     "b": np.ascontiguousarray(b_ada[None, i * CW:(i + 1) * CW])})
    res = run(build_cond(), maps)
    return np.concatenate([r["cond"] for r in res], axis=1)


def mod_off(layer, sub, which):
    return ((layer * 2 + sub) * 3 + which) * D_MODEL


def fm(v):
    return np.ascontiguousarray(v.reshape(-1, 128).T)


DH = 512
GT = 512
NG = SEQ // GT


def build_mlstm_front(SEQ=SEQ):
    NG = SEQ // GT
    nc = new_nc()
    fw = FW(nc)
    xT = fw.dram_in("xT", [D_MODEL, SEQ], F32)
    modp = fw.dram_in("modp", [128, 8, 2], F32)
    w_in = fw.dram_in("w_in", [D_MODEL, 2 * DH], F32)
    convp = fw.dram_in("convp", [128, 4, 6], F32)
    wq = fw.dram_in("wq", [DH, DH], F32)
    wk = fw.dram_in("wk", [DH, DH], F32)
    wv = fw.dram_in("wv", [DH, DH], F32)
    wif = fw.dram_in("wif", [DH, 8], F32)
    qT_o = fw.dram_out("qT", [DH, SEQ], BF16)
    kT_o = fw.dram_out("kT", [DH, SEQ], BF16)
    k_o = fw.dram_out("k", [SEQ, DH], BF16)
    v_o = fw.dram_out("v", [SEQ, DH], BF16)
    sxc_o = fw.dram_out("sxcT", [DH, SEQ], BF16)
    sz_o = fw.dram_out("szT", [DH, SEQ], BF16)
    gp_o = fw.dram_out("gp", [8, SEQ], F32)

    modp_sb = fw.sbuf([128, 8, 2], F32, "modp_sb")
    sc1 = fw.sbuf([128, 8], F32, "sc1")
    convp_sb = fw.sbuf([128, 4, 6], F32, "convp_sb")
    w_in_sb = fw.sbuf([128, 8, 2 * DH], BF16, "w_in_sb")
    wq_sb = fw.sbuf([128, 4, DH], BF16, "wq_sb")
    wk_sb = fw.sbuf([128, 4, DH], BF16, "wk_sb")
    wv_sb = fw.sbuf([128, 4, DH], BF16, "wv_sb")
    wif_sb = fw.sbuf([128, 4, 8], BF16, "wif_sb")
    fw.dma("sp", modp_sb[:], modp[:], writes=[modp_sb])
    fw.dma("sp", convp_sb[:], convp[:], writes=[convp_sb])
    for ch in range(8):
        fw.dma("pool", w_in_sb[:, ch, :], w_in[ch * 128:(ch + 1) * 128, :], writes=[w_in_sb])
    for ch in range(4):
        fw.dma("pool", wq_sb[:, ch, :], wq[ch * 128:(ch + 1) * 128, :], writes=[wq_sb])
        fw.dma("pool", wk_sb[:, ch, :], wk[ch * 128:(ch + 1) * 128, :], writes=[wk_sb])
        fw.dma("pool", wv_sb[:, ch, :], wv[ch * 128:(ch + 1) * 128, :], writes=[wv_sb])
        fw.dma("pool", wif_sb[:, ch, :], wif[ch * 128:(ch + 1) * 128, :], writes=[wif_sb])
    fw.op("dve", lambda: nc.vector.tensor_scalar_add(sc1[:], modp_sb[:, :, 1], 1.0), reads=[modp_sb], writes=[sc1])

    x_r = fw.ring(2, [128, 8, GT], F32, "x_g")
    h_r = fw.ring(2, [128, 8, GT], BF16, "h_g")
    xm_r = fw.ring(2, [128, 4, GT + 4], BF16, "xm_g")
    sz_r = fw.ring(2, [128, 4, GT], BF16, "sz_g")
    acc_r = fw.ring(2, [128, GT], F32, "acc")
    xc_r = fw.ring(2, [128, 4, GT], BF16, "xc_g")
    sxc_r = fw.ring(2, [128, 4, GT], BF16, "sxc_g")
    q_r = fw.ring(2, [128, 4, GT], BF16, "q_g")
    kT_r = fw.ring(2, [128, 4, GT], BF16, "kT_g")
    k_r = fw.ring(2, [128, 4, DH], BF16, "k_g")
    v_r = fw.ring(2, [128, 4, DH], BF16, "v_g")
    gp_r = fw.ring(2, [8, GT], F32, "gp_g")
    ps = fw.ring(7, [128, 512], F32, "ps", psum=True)
    xT_v = xT[:].rearrange("(c p) t -> p c t", p=128)
    xm_prev = None
    xg_next = x_r.next()
    fw.dma("sp", xg_next[:], xT_v[:, :, 0:GT], writes=[xg_next])
    for g in range(NG):
        ts = slice(g * GT, (g + 1) * GT)
        xg = xg_next
        if g + 1 < NG:
            xg_next = x_r.next()
            fw.dma("sp", xg_next[:], xT_v[:, :, (g + 1) * GT:(g + 2) * GT], writes=[xg_next])
        hg = h_r.next()
        for ch in range(8):
            fw.op("dve", lambda ch=ch: nc.vector.tensor_scalar(
                hg[:, ch, :], xg[:, ch, :], sc1[:, ch:ch + 1], modp_sb[:, ch, 0:1], ALU.mult, ALU.add),
                reads=[xg, sc1, modp_sb], writes=[hg])
        xm = xm_r.next()
        sz = sz_r.next()
        if xm_prev is None:
            fw.op("pool", lambda: nc.gpsimd.memset(xm[:, :, 0:4], 0.0), writes=[xm])
        else:
            fw.op("pool", lambda xp=xm_prev: nc.gpsimd.tensor_copy(xm[:, :, 0:4], xp[:, :, GT:GT + 4]),
                  reads=[xm_prev], writes=[xm])
        for m in range(4):
            p = ps.next()
            fw.mm(p[:], [(w_in_sb[:, kk, m * 128:(m + 1) * 128], hg[:, kk, :]) for kk in range(8)],
                  reads=[w_in_sb, hg], writes=[p])
            fw.op("dve", lambda p=p, m=m: nc.vector.tensor_copy(xm[:, m, 4:4 + GT], p[:]), reads=[p], writes=[xm])
        for m in range(4):
            p = ps.next()
            fw.mm(p[:], [(w_in_sb[:, kk, DH + m * 128:DH + (m + 1) * 128], hg[:, kk, :]) for kk in range(8)],
                  reads=[w_in_sb, hg], writes=[p])
            fw.op("act", lambda p=p, m=m: nc.scalar.activation(sz[:, m, :], p[:], AF.Silu), reads=[p], writes=[sz])
        fw.dma("sp", sz_o[:].rearrange("(c p) t -> p c t", p=128)[:, :, ts], sz[:], reads=[sz], writes=[sz_o])
        xc = xc_r.next()
        sxc = sxc_r.next()
        for m in range(4):
            acc = acc_r.next()
            fw.op("dve", lambda m=m, acc=acc: nc.vector.tensor_scalar(
                acc[:], xm[:, m, 4:4 + GT], convp_sb[:, m, 3:4], convp_sb[:, m, 4:5], ALU.mult, ALU.add),
                reads=[xm, convp_sb], writes=[acc])
            for j in range(3):
                fw.op("dve", lambda m=m, acc=acc, j=j: nc.vector.scalar_tensor_tensor(
                    acc[:], xm[:, m, 1 + j:1 + j + GT], convp_sb[:, m, j:j + 1], acc[:], ALU.mult, ALU.add),
                    reads=[xm, convp_sb, acc], writes=[acc])
            fw.op("act", lambda m=m, acc=acc: nc.scalar.activation(xc[:, m, :], acc[:], AF.Silu),
                  reads=[acc], writes=[xc])
            fw.op("pool", lambda m=m: nc.gpsimd.tensor_scalar(
                sxc[:, m, :], xc[:, m, :], convp_sb[:, m, 5:6], None, ALU.mult),
                reads=[xc, convp_sb], writes=[sxc])
        fw.dma("sp", sxc_o[:].rearrange("(c p) t -> p c t", p=128)[:, :, ts], sxc[:], reads=[sxc], writes=[sxc_o])
        qg = q_r.next()
        kTg = kT_r.next()
        for m in range(4):
            p = ps.next()
            fw.mm(p[:], [(wq_sb[:, kk, m * 128:(m + 1) * 128], xc[:, kk, :]) for kk in range(4)],
                  reads=[wq_sb, xc], writes=[p])
            fw.op("act", lambda p=p, m=m: nc.scalar.copy(qg[:, m, :], p[:]), reads=[p], writes=[qg])
        for m in range(4):
            p = ps.next()
            fw.mm(p[:], [(wk_sb[:, kk, m * 128:(m + 1) * 128], xc[:, kk, :]) for kk in range(4)],
                  reads=[wk_sb, xc], writes=[p])
            fw.op("act", lambda p=p, m=m: nc.scalar.mul(kTg[:, m, :], p[:], DH ** -0.5), reads=[p], writes=[kTg])
        fw.dma("act", qT_o[:].rearrange("(c p) t -> p c t", p=128)[:, :, ts], qg[:], reads=[qg], writes=[qT_o])
        fw.dma("act", kT_o[:].rearrange("(c p) t -> p c t", p=128)[:, :, ts], kTg[:], reads=[kTg], writes=[kT_o])
        kg = k_r.next()
        vg = v_r.next()
        for tt in range(4):
            p = ps.next()
            fw.mm(p[:], [(xc[:, kk, tt * 128:(tt + 1) * 128], wk_sb[:, kk, :]) for kk in range(4)],
                  reads=[wk_sb, xc], writes=[p])
            fw.op("act", lambda p=p, tt=tt: nc.scalar.mul(kg[:, tt, :], p[:], DH ** -0.5), reads=[p], writes=[kg])
            p = ps.next()
            fw.mm(p[:], [(xm[:, kk, 4 + tt * 128:4 + (tt + 1) * 128], wv_sb[:, kk, :]) for kk in range(4)],
                  reads=[wv_sb, xm], writes=[p])
            fw.op("dve", lambda p=p, tt=tt: nc.vector.tensor_copy(vg[:, tt, :], p[:]), reads=[p], writes=[vg])
        fw.dma("sp", k_o[ts, :].rearrange("(tt p) f -> p tt f", p=128), kg[:], reads=[kg], writes=[k_o])
        fw.dma("sp", v_o[ts, :].rearrange("(tt p) f -> p tt f", p=128), vg[:], reads=[vg], writes=[v_o])
        p = ps.next()
        fw.mm(p[0:8, :], [(wif_sb[:, kk, :], xc[:, kk, :]) for kk in range(4)], reads=[wif_sb, xc], writes=[p])
        gpg = gp_r.next()
        fw.op("dve", lambda p=p: nc.vector.tensor_copy(gpg[:], p[0:8, :]), reads=[p], writes=[gpg])
        fw.dma("sp", gp_o[:, ts], gpg[:], reads=[gpg], writes=[gp_o])
        xm_prev = xm
    fw.wait_all("sp", [qT_o, kT_o, k_o, v_o, sxc_o, sz_o, gp_o])
    fw.wait_all("act", [qT_o, kT_o, k_o, v_o, sxc_o, sz_o, gp_o])
    fw.close()
    return nc


def run_mlstm_front(inp, cond):
    maps = []
    for core in range(NCORES):
        b, hd = divmod(core, 4)
        xT = np.ascontiguousarray(inp["x"][b].T)
        shift = cond[b, mod_off(0, 0, 0):mod_off(0, 0, 0) + D_MODEL]
        scale = cond[b, mod_off(0, 0, 1):mod_off(0, 0, 1) + D_MODEL]
        modp = np.ascontiguousarray(np.stack([fm(shift), fm(scale)], axis=-1))
        w_in = inp["a_w_in"][0]
        w_in_c = np.ascontiguousarray(np.concatenate(
            [w_in[:, hd * DH:(hd + 1) * DH], w_in[:, 2048 + hd * DH:2048 + (hd + 1) * DH]], axis=1))
        sl = slice(hd * DH, (hd + 1) * DH)
        cw = inp["a_conv_w"][0][:, sl]
        rows = [cw[j] for j in range(4)] + [inp["a_conv_b"][0][sl], inp["a_skip"][0][sl]]
        convp = np.ascontiguousarray(np.stack([r.reshape(4, 128).T for r in rows], axis=-1))
        maps.append({"xT": xT, "modp": modp, "w_in": w_in_c, "convp": convp,
                     "wq": np.ascontiguousarray(inp["a_wq"][0][hd]), "wk": np.ascontiguousarray(inp["a_wk"][0][hd]),
                     "wv": np.ascontiguousarray(inp["a_wv"][0][hd]),
                     "wif": np.ascontiguousarray(inp["a_w_if"][0][sl, :])})
    return run(build_mlstm_front(), maps)


LCH = 128
NCH = SEQ // LCH


def build_mlstm_core(SEQ=SEQ):
    NCH = SEQ // LCH
    nc = new_nc()
    fw = FW(nc)
    qT = fw.dram_in("qT", [DH, SEQ], BF16)
    kT = fw.dram_in("kT", [DH, SEQ], BF16)
    k_i = fw.dram_in("k", [SEQ, DH], BF16)
    v_i = fw.dram_in("v", [SEQ, DH], BF16)
    sxc_i = fw.dram_in("sxcT", [DH, SEQ], BF16)
    sz_i = fw.dram_in("szT", [DH, SEQ], BF16)
    gpi = fw.dram_in("gpi", [4, SEQ], F32)
    gpf = fw.dram_in("gpf", [4, SEQ], F32)
    bif = fw.dram_in("bif", [1, 2], F32)
    gng = fw.dram_in("gng", [128, 4], F32)
    cst = fw.dram_in("cst", [128, 256], F32)
    hg_o = fw.dram_out("hgT", [DH, SEQ], BF16)

    cst_sb = fw.sbuf([128, 256], F32, "cst_sb")
    ident_b = fw.sbuf([128, 128], BF16, "ident_b")
    ones_f = fw.sbuf([128, 128], F32, "ones_f")
    ones_b = fw.sbuf([128, 1], BF16, "ones_b")
    gng_sb = fw.sbuf([128, 4], F32, "gng_sb")
    bif_sb = fw.sbuf([1, 2], F32, "bif_sb")
    cols = fw.sbuf([128, 3, NCH], F32, "cols")
    ulast = fw.sbuf([128, NCH], F32, "ulast")
    u0 = fw.sbuf([128, NCH], F32, "u0")
    wint = fw.sbuf([128, NCH], F32, "wint")
    emt = fw.sbuf([128, NCH], F32, "emt")
    wsc = fw.sbuf([128, NCH], F32, "wsc")
    dec = fw.sbuf([128, NCH], F32, "dec")
    ident_f = cst_sb
    fw.dma("sp", cst_sb[:], cst[:], writes=[cst_sb])
    fw.dma("pool", ident_b[:], cst[:, 0:128], writes=[ident_b])
    fw.dma("sp", gng_sb[:], gng[:], writes=[gng_sb])
    fw.dma("sp", bif_sb[:], bif[:], writes=[bif_sb])
    fw.op("dve", lambda: nc.vector.memset(ones_f[:], 1.0), writes=[ones_f])
    fw.op("dve", lambda: nc.vector.memset(ones_b[:], 1.0), writes=[ones_b])

    mark = fw.scope_begin()
    g4 = fw.sbuf([4, SEQ], F32, "g4")
    irow = fw.sbuf([1, SEQ], F32, "irow")
    frow = fw.sbuf([1, SEQ], F32, "frow")
    trow = fw.sbuf([1, SEQ], F32, "trow")
    brow = fw.sbuf([1, SEQ], F32, "brow")
    gps = fw.ring(4, [128, 512], F32, "gps", psum=True)
    for src, dst, bi in ((gpi, irow, 0), (gpf, frow, 1)):
        fw.dma("sp", g4[:], src[:], writes=[g4])
        for j in range(SEQ // 512):
            p = gps.next()
            fw.mm(p[0:1, :], [(ones_f[0:4, 0:1], g4[:, j * 512:(j + 1) * 512])], reads=[ones_f, g4], writes=[p])
            fw.op("act", lambda p=p, j=j, dst=dst, bi=bi: nc.scalar.activation(
                dst[:, j * 512:(j + 1) * 512], p[0:1, :], AF.Identity, bias=bif_sb[:, bi:bi + 1]),
                reads=[p, bif_sb], writes=[dst])
    fw.op("act", lambda: nc.scalar.activation(trow[:], frow[:], AF.Abs), reads=[frow], writes=[trow])
    fw.op("act", lambda: nc.scalar.activation(trow[:], trow[:], AF.Exp, scale=-1.0), reads=[trow], writes=[trow])
    fw.op("act", lambda: nc.scalar.activation(trow[:], trow[:], AF.Ln, bias=1.0), reads=[trow], writes=[trow])
    fw.op("dve", lambda: nc.vector.scalar_tensor_tensor(frow[:], frow[:], 0.0, trow[:], ALU.min, ALU.subtract),
          reads=[frow, trow], writes=[frow])
    one_bc = ones_f[0:1, 0:1].to_broadcast([1, SEQ])
    fw.op("dve", lambda: nc.vector.tensor_tensor_scan(brow[:], one_bc, frow[:], 0.0, ALU.mult, ALU.add),
          reads=[ones_f, frow], writes=[brow])
    fw.op("dve", lambda: nc.vector.tensor_sub(irow[:], irow[:], brow[:]), reads=[irow, brow], writes=[irow])
    fw.op("dve", lambda: nc.vector.tensor_tensor_scan(trow[:], one_bc, irow[:], 0.0, ALU.mult, ALU.max),
          reads=[ones_f, irow], writes=[trow])
    fw.op("dve", lambda: nc.vector.tensor_add(brow[:], brow[:], trow[:]), reads=[brow, trow], writes=[brow])
    cp = gps.next()
    for ai, row in enumerate((irow, trow, brow)):
        for c in range(NCH):
            fw.op("pe", lambda ai=ai, row=row, c=c: nc.tensor.transpose(
                cp[:, ai * NCH + c:ai * NCH + c + 1], row[0:1, c * LCH:(c + 1) * LCH], ident_f[0:1, 0:1]),
                reads=[row, cst_sb], writes=[cp], inc=(c == NCH - 1))
    fw.op("dve", lambda: nc.vector.tensor_copy(cols[:].rearrange("p a c -> p (a c)"), cp[:, 0:3 * NCH]),
          reads=[cp], writes=[cols])
    bp = gps.next()
    fw.mm(bp[:, 0:NCH], [(ones_f[0:1, :], trow[0:1, LCH - 1::LCH])], reads=[ones_f, trow], writes=[bp])
    fw.op("dve", lambda: nc.vector.tensor_copy(ulast[:], bp[:, 0:NCH]), reads=[bp], writes=[ulast])
    fw.op("dve", lambda: nc.vector.memset(u0[:, 0:1], 0.0), writes=[u0])
    fw.op("dve", lambda: nc.vector.tensor_copy(u0[:, 1:NCH], ulast[:, 0:NCH - 1]), reads=[ulast], writes=[u0])
    tmpc = fw.sbuf([128, NCH], F32, "tmpc")
    fw.op("dve", lambda: nc.vector.tensor_sub(tmpc[:], u0[:], cols[:, 1, :]), reads=[u0, cols], writes=[tmpc])
    fw.op("act", lambda: nc.scalar.activation(wint[:], tmpc[:], AF.Exp), reads=[tmpc], writes=[wint])
    fw.op("act", lambda: nc.scalar.activation(emt[:], cols[:, 2, :], AF.Exp, scale=-1.0), reads=[cols], writes=[emt])
    fw.op("dve", lambda: nc.vector.tensor_sub(tmpc[:], cols[:, 0, :], ulast[:]), reads=[ulast, cols], writes=[tmpc])
    fw.op("act", lambda: nc.scalar.activation(wsc[:], tmpc[:], AF.Exp), reads=[tmpc], writes=[wsc])
    fw.op("dve", lambda: nc.vector.tensor_sub(tmpc[:], u0[:], ulast[:]), reads=[u0, ulast], writes=[tmpc])
    fw.op("act", lambda: nc.scalar.activation(dec[:], tmpc[:], AF.Exp), reads=[tmpc], writes=[dec])
    fw.scope_end(mark)

    GC = 4
    q_r = fw.ring(2, [128, 4, GC * LCH], BF16, "q_c")
    kT_r = fw.ring(2, [128, 4, GC * LCH], BF16, "kT_c")
    sxc_r = fw.ring(2, [128, 4, GC * LCH], BF16, "sxc_c")
    sz_r = fw.ring(2, [128, 4, GC * LCH], BF16, "sz_c")
    k_r = fw.ring(2, [128, GC, DH], BF16, "k_c")
    v_r = fw.ring(2, [128, GC, DH], BF16, "v_c")
    hg_r = fw.ring(2, [128, 4, GC * LCH], BF16, "hg_c")
    Cm = fw.sbuf([128, 4, DH], F32, "Cm")
    nm = fw.sbuf([128, 4], F32, "nm")
    Cb = fw.sbuf([128, 4, DH], BF16, "Cb")
    nb = fw.sbuf([128, 4], BF16, "nb")
    diag = fw.ring(2, [128, 128], F32, "diag")
    DT = fw.ring(2, [128, 128], F32, "DT")
    swT = fw.ring(2, [128, 128], BF16, "swT")
    kw = fw.ring(2, [128, DH], BF16, "kw")
    dsb = fw.ring(2, [128, 8], F32, "dsb")
    inter_sb = fw.ring(2, [128, DH], F32, "inter_sb")
    h_sb = fw.ring(2, [128, DH], F32, "h_sb")
    hn_sb = fw.ring(2, [128, DH], BF16, "hn_sb")
    st = fw.ring(2, [128, 8], F32, "st")
    a_sb = fw.ring(2, [128, 4, LCH], F32, "a_sb")
    pA = fw.psum([128, 512], F32, "pA")
    pB = fw.psum([128, 512], F32, "pB")
    pC = fw.psum([128, 512], F32, "pC")
    pT = fw.psum([128, 4, LCH], BF16, "pT")
    pD = [fw.psum([128, 512], F32, f"pD{m}") for m in range(2)]
    pN = fw.psum([128, 8], F32, "pN")
    fw.op("dve", lambda: nc.vector.memset(Cm[:], 0.0), writes=[Cm])
    fw.op("dve", lambda: nc.vector.memset(nm[:], 0.0), writes=[nm])
    fw.op("pool", lambda: nc.gpsimd.memset(Cb[:], 0.0), writes=[Cb])
    fw.op("pool", lambda: nc.gpsimd.memset(nb[:], 0.0), writes=[nb])
    fmv = lambda ap: ap[:].rearrange("(c p) t -> p c t", p=128)
    def load_group(g):
        ts = slice(g * GC * LCH, (g + 1) * GC * LCH)
        bufs = (q_r.next(), kT_r.next(), sxc_r.next(), sz_r.next(), k_r.next(), v_r.next())
        qg, kTg, sxg, szg, kg, vg = bufs
        fw.dma("sp", qg[:], fmv(qT)[:, :, ts], writes=[qg])
        fw.dma("sp", kTg[:], fmv(kT)[:, :, ts], writes=[kTg])
        fw.dma("sp", kg[:], k_i[ts, :].rearrange("(tt p) f -> p tt f", p=128), writes=[kg])
        fw.dma("sp", vg[:], v_i[ts, :].rearrange("(tt p) f -> p tt f", p=128), writes=[vg])
        fw.dma("sp", sxg[:], fmv(sxc_i)[:, :, ts], writes=[sxg])
        fw.dma("sp", szg[:], fmv(sz_i)[:, :, ts], writes=[szg])
        return bufs

    nxt = load_group(0)
    for g in range(NCH // GC):
        ts = slice(g * GC * LCH, (g + 1) * GC * LCH)
        qg, kTg, sxg, szg, kg, vg = nxt
        hgg = hg_r.next()
        if g + 1 < NCH // GC:
            nxt = load_group(g + 1)
        for cc in range(GC):
            c = g * GC + cc
            cs = slice(cc * LCH, (cc + 1) * LCH)
            fw.mm(pC[:], [(qg[:, m, cs], Cb[:, m, :]) for m in range(4)], reads=[qg, Cb], writes=[pC])
            fw.mm(pA[:, 257:258], [(qg[:, m, cs], nb[:, m:m + 1]) for m in range(4)], reads=[qg, nb], writes=[pA])
            kwt = kw.next()
            fw.op("pool", lambda kwt=kwt, c=c, cc=cc: nc.gpsimd.tensor_scalar(
                kwt[:], kg[:, cc, :], wsc[:, c:c + 1], None, ALU.mult), reads=[kg, wsc], writes=[kwt])
            for m in range(4):
                fw.mm(pN[:, m:m + 1], [(kwt[:, m * 128:(m + 1) * 128], ones_b[:])], reads=[kwt, ones_b], writes=[pN])
            for m in range(4):
                pd = pD[m % 2]
                fw.mm(pd[:], [(kwt[:, m * 128:(m + 1) * 128], vg[:, cc, :])], reads=[kwt, vg], writes=[pd])
                fw.op("dve", lambda m=m, c=c, pd=pd: nc.vector.scalar_tensor_tensor(
                    Cm[:, m, :], Cm[:, m, :], dec[:, c:c + 1], pd[:], ALU.mult, ALU.add),
                    reads=[Cm, dec, pd], writes=[Cm])
            fw.op("dve", lambda c=c: nc.vector.scalar_tensor_tensor(
                nm[:], nm[:], dec[:, c:c + 1], pN[:, 0:4], ALU.mult, ALU.add), reads=[nm, dec, pN], writes=[nm])
            fw.mm(pA[:, 0:128], [(kTg[:, m, cs], qg[:, m, cs]) for m in range(4)], reads=[kTg, qg], writes=[pA])
            dg = diag.next()
            fw.op("dve", lambda dg=dg, c=c: nc.vector.tensor_scalar(
                dg[:], ident_f[:, 0:128], cols[:, 1, c:c + 1], None, ALU.mult), reads=[cst_sb, cols], writes=[dg])
            fw.mm(pA[:, 128:256], [(ones_f[:], dg[:]), (ident_f[:, 0:128], cst_sb[:, 128:256])],
                  reads=[ones_f, dg, cst_sb], writes=[pA])
            dt = DT.next()
            fw.op("act", lambda dt=dt, c=c: nc.scalar.activation(
                dt[:], pA[:, 128:256], AF.Exp, bias=cols[:, 0, c:c + 1], scale=-1.0), reads=[pA, cols], writes=[dt])
            sw = swT.next()
            fw.op("dve", lambda sw=sw, dt=dt: nc.vector.tensor_mul(sw[:], pA[:, 0:128], dt[:]), reads=[pA, dt], writes=[sw])
            fw.mm(pB[:], [(sw[:], vg[:, cc, :])], reads=[sw, vg], writes=[pB])
            fw.mm(pA[:, 256:257], [(sw[:], ones_b[:])], reads=[sw, ones_b], writes=[pA])
            fw.op("act", lambda: nc.scalar.copy(Cb[:], Cm[:]), reads=[Cm], writes=[Cb])
            fw.op("act", lambda: nc.scalar.copy(nb[:], nm[:]), reads=[nm], writes=[nb])
            d = dsb.next()
            fw.op("dve", lambda d=d: nc.vector.tensor_copy(d[:, 0:2], pA[:, 256:258]), reads=[pA], writes=[d])
            fw.op("dve", lambda d=d, c=c: nc.vector.scalar_tensor_tensor(
                d[:, 2:3], d[:, 1:2], wint[:, c:c + 1], d[:, 0:1], ALU.mult, ALU.add), reads=[d, wint], writes=[d])
            fw.op("dve", lambda d=d: nc.vector.tensor_scalar(d[:, 6:7], d[:, 2:3], -1.0, None, ALU.mult), reads=[d], writes=[d])
            fw.op("dve", lambda d=d: nc.vector.tensor_max(d[:, 6:7], d[:, 6:7], d[:, 2:3]), reads=[d], writes=[d])
            fw.op("dve", lambda d=d, c=c: nc.vector.tensor_max(d[:, 3:4], d[:, 6:7], emt[:, c:c + 1]),
                  reads=[d, emt], writes=[d])
            fw.op("dve", lambda d=d: nc.vector.reciprocal(d[:, 4:5], d[:, 3:4]), reads=[d], writes=[d])
            fw.op("dve", lambda d=d, c=c: nc.vector.tensor_mul(d[:, 5:6], d[:, 4:5], wint[:, c:c + 1]),
                  reads=[d, wint], writes=[d])
            isb = inter_sb.next()
            fw.op("act", lambda isb=isb, d=d: nc.scalar.activation(isb[:], pC[:], AF.Copy, scale=d[:, 5:6]),
                  reads=[pC, d], writes=[isb])
            h = h_sb.next()
            fw.op("dve", lambda h=h, d=d, isb=isb: nc.vector.scalar_tensor_tensor(
                h[:], pB[:], d[:, 4:5], isb[:], ALU.mult, ALU.add), reads=[pB, d, isb], writes=[h])
            s = st.next()
            fw.op("dve", lambda s=s, h=h: nc.vector.bn_stats(s[:, 0:6], h[:]), reads=[h], writes=[s])
            fw.op("dve", lambda s=s: nc.vector.bn_aggr(s[:, 6:8], s[:, 0:6]), reads=[s], writes=[s])
            fw.op("act", lambda s=s: nc.scalar.activation(s[:, 7:8], s[:, 7:8], AF.Ln, bias=LN_EPS), reads=[s], writes=[s])
            fw.op("act", lambda s=s: nc.scalar.activation(s[:, 7:8], s[:, 7:8], AF.Exp, scale=-0.5), reads=[s], writes=[s])
            hn = hn_sb.next()
            fw.op("dve", lambda hn=hn, h=h, s=s: nc.vector.tensor_scalar(
                hn[:], h[:], s[:, 6:7], s[:, 7:8], ALU.subtract, ALU.mult), reads=[h, s], writes=[hn])
            for m in range(4):
                fw.op("pe", lambda m=m, hn=hn: nc.tensor.transpose(pT[:, m, :], hn[:, m * 128:(m + 1) * 128], ident_b[:]),
                      reads=[hn, ident_b], writes=[pT], inc=(m == 3))
            a = a_sb.next()
            for m in range(4):
                fw.op("dve", lambda m=m, a=a: nc.vector.scalar_tensor_tensor(
                    a[:, m, :], pT[:, m, :], gng_sb[:, m:m + 1], sxg[:, m, cs], ALU.mult, ALU.add),
                    reads=[pT, gng_sb, sxg], writes=[a])
            fw.op("pool", lambda a=a: nc.gpsimd.tensor_mul(hgg[:, :, cs], a[:], szg[:, :, cs]), reads=[a, szg], writes=[hgg])
        fw.dma("sp", fmv(hg_o)[:, :, ts], hgg[:], reads=[hgg], writes=[hg_o])
    fw.wait_all("sp", [hg_o])
    fw.close()
    return nc


def mlstm_consts():
    cst = np.zeros((128, 256), np.float32)
    cst[:, :128] = np.eye(128, dtype=np.float32)
    sp, t = np.meshgrid(np.arange(128), np.arange(128), indexing="ij")
    cst[:, 128:] = np.where(sp > t, BIG, 0.0)
    return cst


def run_mlstm_core(inp, front):
    maps = []
    cst = mlstm_consts()
    for core in range(NCORES):
        b, hd = divmod(core, 4)
        f = front[core]
        gpi = np.ascontiguousarray(np.stack([front[b * 4 + j]["gp"][hd] for j in range(4)]))
        gpf = np.ascontiguousarray(np.stack([front[b * 4 + j]["gp"][4 + hd] for j in range(4)]))
        b_if = inp["a_b_if"][0]
        maps.append({"qT": f["qT"], "kT": f["kT"], "k": f["k"], "v": f["v"], "sxcT": f["sxcT"], "szT": f["szT"],
                     "gpi": gpi, "gpf": gpf, "bif": np.array([[b_if[hd], b_if[4 + hd]]], np.float32),
                     "gng": np.ascontiguousarray(inp["a_gn_g"][0][hd * DH:(hd + 1) * DH].reshape(4, 128).T),
                     "cst": cst})
    return run(build_mlstm_core(), maps)


NT = 16
NE = 16
DFF = 512


def load_rows(fw, nc, rows_d, idxs, name, plus1=()):
    t = fw.sbuf([128, len(idxs), D_MODEL], F32, name)
    for j, i in enumerate(idxs):
        fw.dma("sp" if j % 2 == 0 else "act", t[:, j, :], rows_d[i:i + 1, :].to_broadcast([128, D_MODEL]), writes=[t])
    for j, i in enumerate(idxs):
        if i in plus1:
            fw.op("pool", lambda j=j: nc.gpsimd.tensor_scalar_add(t[:, j, :], t[:, j, :], 1.0), reads=[t], writes=[t])
    return t


def ln_tile(fw, nc, r, st, rows, ig, ib, x_out):
    fw.op("dve", lambda: nc.vector.bn_stats(st[:, 0:6], r[:, 0:512]), reads=[r], writes=[st])
    fw.op("dve", lambda: nc.vector.bn_stats(st[:, 6:12], r[:, 512:1024]), reads=[r], writes=[st])
    fw.op("dve", lambda: nc.vector.bn_aggr(st[:, 12:14], st[:, 0:12]), reads=[st], writes=[st])
    fw.op("act", lambda: nc.scalar.activation(st[:, 13:14], st[:, 13:14], AF.Ln, bias=LN_EPS), reads=[st], writes=[st])
    fw.op("act", lambda: nc.scalar.activation(st[:, 13:14], st[:, 13:14], AF.Exp, scale=-0.5), reads=[st], writes=[st])
    fw.op("dve", lambda: nc.vector.tensor_scalar(r[:], r[:], st[:, 12:13], st[:, 13:14], ALU.subtract, ALU.mult),
          reads=[r, st], writes=[r])
    fw.op("pool", lambda: nc.gpsimd.tensor_mul(r[:], r[:], rows[:, ig, :]), reads=[r, rows], writes=[r])
    fw.op("dve", lambda: nc.vector.tensor_add(x_out[:], r[:], rows[:, ib, :]), reads=[r, rows], writes=[x_out])


def mod_tile(fw, nc, x_in, rows, isc, ish, h_out):
    fw.op("pool", lambda: nc.gpsimd.tensor_mul(h_out[:], x_in[:], rows[:, isc, :]), reads=[x_in, rows], writes=[h_out])
    fw.op("dve", lambda: nc.vector.tensor_add(h_out[:], h_out[:], rows[:, ish, :]), reads=[h_out, rows], writes=[h_out])


def transpose_tile(fw, nc, h32, ident, ps2, outs):
    for k in range(8):
        fw.op("pe", lambda k=k: nc.tensor.transpose(ps2[:, k * 128:(k + 1) * 128], h32[:, k * 128:(k + 1) * 128],
                                                    ident[:, 0:128]),
              reads=[h32, ident], writes=[ps2], inc=(k == 7))
    src = ps2[:].rearrange("p (k t) -> p k t", k=8)
    for e, dst, dbuf in outs:
        if e == "act":
            fw.op("act", lambda dst=dst: nc.scalar.copy(dst, src), reads=[ps2], writes=[dbuf])
        else:
            fw.op("dve", lambda dst=dst: nc.vector.tensor_copy(dst, src), reads=[ps2], writes=[dbuf])


def router_tile(fw, nc, hT32, wr_sb, brow, psl, rt, cw_out):
    fw.mm(psl[:, 0:16], [(hT32[:, k, :], wr_sb[:, k, :]) for k in range(8)], reads=[hT32, wr_sb], writes=[psl])
    V = nc.vector
    o = lambda fn, reads=(), writes=(): fw.op("dve", fn, reads=list(reads) + [rt], writes=list(writes) + [rt])
    o(lambda: V.reduce_max(rt[:, 0:1], psl[:, 0:16], AX.X), reads=[psl])
    o(lambda: V.tensor_scalar(rt[:, 1:2], rt[:, 0:1], -1.0, None, ALU.mult))
    fw.op("act", lambda: nc.scalar.activation(rt[:, 32:48], psl[:, 0:16], AF.Exp, bias=rt[:, 1:2], accum_out=rt[:, 2:3]),
          reads=[psl, rt], writes=[rt])
    o(lambda: V.reciprocal(rt[:, 3:4], rt[:, 2:3]))
    o(lambda: V.tensor_scalar(rt[:, 32:48], rt[:, 32:48], rt[:, 3:4], None, ALU.mult))
    o(lambda: V.tensor_add(rt[:, 48:64], rt[:, 32:48], brow[:]), reads=[brow])
    g3 = lambda a: rt[:, a:a + 16].rearrange("p (g e) -> p g e", g=4)
    bc4 = lambda a: rt[:, a:a + 4].unsqueeze(2).to_broadcast([128, 4, 4])
    o(lambda: V.tensor_reduce(rt[:, 4:8], g3(48), AX.X, ALU.max))
    o(lambda: V.tensor_tensor(g3(64), g3(48), bc4(4), ALU.is_equal))
    o(lambda: V.scalar_tensor_tensor(rt[:, 64:80], rt[:, 64:80], -BIG, rt[:, 48:64], ALU.mult, ALU.add))
    o(lambda: V.tensor_reduce(rt[:, 8:12], g3(64), AX.X, ALU.max))
    o(lambda: V.tensor_add(rt[:, 8:12], rt[:, 8:12], rt[:, 4:8]))
    o(lambda: V.reduce_max(rt[:, 12:13], rt[:, 8:12], AX.X))
    o(lambda: V.tensor_scalar(rt[:, 13:17], rt[:, 8:12], rt[:, 12:13], None, ALU.is_equal))
    o(lambda: V.tensor_scalar(rt[:, 13:17], rt[:, 13:17], -1.0, BIG, ALU.add, ALU.mult))
    o(lambda: V.tensor_tensor(g3(64), g3(48), bc4(13), ALU.add))
    o(lambda: V.reduce_max(rt[:, 17:18], rt[:, 64:80], AX.X))
    o(lambda: V.tensor_scalar(rt[:, 80:96], rt[:, 64:80], rt[:, 17:18], None, ALU.is_equal))
    o(lambda: V.scalar_tensor_tensor(rt[:, 96:112], rt[:, 80:96], -BIG, rt[:, 64:80], ALU.mult, ALU.add))
    o(lambda: V.reduce_max(rt[:, 18:19], rt[:, 96:112], AX.X))
    o(lambda: V.tensor_scalar(rt[:, 96:112], rt[:, 96:112], rt[:, 18:19], None, ALU.is_equal))
    o(lambda: V.tensor_add(rt[:, 80:96], rt[:, 80:96], rt[:, 96:112]))
    o(lambda: V.tensor_mul(rt[:, 112:128], rt[:, 32:48], rt[:, 80:96]))
    o(lambda: V.reduce_sum(rt[:, 19:20], rt[:, 112:128], AX.X))
    o(lambda: V.reciprocal(rt[:, 20:21], rt[:, 19:20]))
    fw.op("dve", lambda: V.tensor_scalar(cw_out, rt[:, 112:128], rt[:, 20:21], None, ALU.mult), reads=[rt], writes=[rt])


def moe_dense(fw, nc, hT, cw, cw_buf, wg_d, wu_d, wd_d, yacc):
    mark = fw.scope_begin()
    wg_r = fw.ring(2, [128, 8, DFF], BF16, "wg")
    wu_r = fw.ring(2, [128, 8, DFF], BF16, "wu")
    wd_r = fw.ring(2, [128, 4, D_MODEL], BF16, "wd")
    sg_r = fw.ring(2, [128, 512], F32, "sg")
    aT_r = fw.ring(2, [128, 4, 512], BF16, "aT")
    psg = fw.ring(2, [128, 512], F32, "psg", psum=True)
    psu = fw.ring(2, [128, 512], F32, "psu", psum=True)
    psd = fw.ring(3, [128, 512], F32, "psd", psum=True)
    wts = {}

    def load_w(e):
        wg, wu, wd = wg_r.next(), wu_r.next(), wd_r.next()
        fw.dma("pool", wg[:], wg_d[e].rearrange("(k p) f -> p k f", p=128), writes=[wg])
        fw.dma("pool", wu[:], wu_d[e].rearrange("(k p) f -> p k f", p=128), writes=[wu])
        fw.dma("pool", wd[:], wd_d[e].rearrange("(k p) f -> p k f", p=128), writes=[wd])
        wts[e] = (wg, wu, wd)

    def gate_up(e, g):
        wg, wu, _ = wts[e]
        gs = slice(g * 512, (g + 1) * 512)
        aT = aT_r.next()
        for m in range(4):
            pg, pu = psg.next(), psu.next()
            fw.mm(pg[:], [(wg[:, k, m * 128:(m + 1) * 128], hT[:, k, gs]) for k in range(8)], reads=[wg, hT], writes=[pg])
            fw.mm(pu[:], [(wu[:, k, m * 128:(m + 1) * 128], hT[:, k, gs]) for k in range(8)], reads=[wu, hT], writes=[pu])
            sg = sg_r.next()
            fw.op("act", lambda sg=sg, pg=pg: nc.scalar.activation(sg[:], pg[:], AF.Silu), reads=[pg], writes=[sg])
            fw.op("dve", lambda sg=sg, pu=pu, m=m, aT=aT: nc.vector.tensor_mul(aT[:, m, :], pu[:], sg[:]),
                  reads=[pu, sg], writes=[aT])
        return aT

    def down(e, g, aT):
        wd = wts[e][2]
        for tt in range(4):
            T = g * 4 + tt
            for half in range(2):
                pd = psd.next()
                fw.mm(pd[:], [(aT[:, m, tt * 128:(tt + 1) * 128], wd[:, m, half * 512:(half + 1) * 512]) for m in range(4)],
                      reads=[aT, wd], writes=[pd])
                ysl = yacc[:, T, half * 512:(half + 1) * 512]
                if e == 0:
                    fw.op("dve", lambda pd=pd, ysl=ysl, T=T, e=e: nc.vector.tensor_scalar(
                        ysl, pd[:], cw[:, T, e:e + 1], None, ALU.mult), reads=[pd, cw_buf], writes=[yacc])
                else:
                    fw.op("dve", lambda pd=pd, ysl=ysl, T=T, e=e: nc.vector.scalar_tensor_tensor(
                        ysl, pd[:], cw[:, T, e:e + 1], ysl, ALU.mult, ALU.add), reads=[pd, cw_buf, yacc], writes=[yacc])

    steps = [(e, g) for e in range(NE) for g in range(NT // 4)]
    load_w(0)
    load_w(1)
    aT_cur = gate_up(*steps[0])
    for k, (e, g) in enumerate(steps):
        aT_next = gate_up(*steps[k + 1]) if k + 1 < len(steps) else None
        down(e, g, aT_cur)
        aT_cur = aT_next
        if g == NT // 4 - 1 and e + 2 < NE:
            load_w(e + 2)
    fw.scope_end(mark)


def mix_ln_phase(fw, nc, y_fn, x_src, x1_d, rows, hT, cw, cw_buf, wr_sb, brow, ident_f, dbg=None, pst=None, psy_n=2):
    x_r = fw.ring(2, [128, D_MODEL], F32, "xa")
    r_r = fw.ring(2, [128, D_MODEL], F32, "ra")
    x1_r = fw.ring(2, [128, D_MODEL], F32, "x1a")
    h_r = fw.ring(2, [128, D_MODEL], F32, "ha")
    hT32_r = fw.ring(2, [128, 8, 128], F32, "hT32")
    st_r = fw.ring(2, [128, 16], F32, "sta")
    rt_r = fw.ring(2, [128, 128], F32, "rt")
    psy = fw.ring(psy_n, [128, D_MODEL], F32, "psy", psum=True)
    if pst is None:
        pst = fw.ring(1, [128, D_MODEL], F32, "pst", psum=True)
    psl = fw.ring(2, [128, 512], F32, "psl", psum=True)
    xt_next = x_r.next()
    fw.dma("sp", xt_next[:], x_src[0:128, :], writes=[xt_next])
    for T in range(NT if dbg is None else 1):
        xt = xt_next
        if T + 1 < NT:
            xt_next = x_r.next()
            fw.dma("sp", xt_next[:], x_src[(T + 1) * 128:(T + 2) * 128, :], writes=[xt_next])
        py = psy.next()
        if dbg == "A0":
            return
        y_fn(T, py)
        r = r_r.next()
        fw.op("dve", lambda r=r, py=py: nc.vector.tensor_mul(r[:], py[:], rows[:, 0, :]), reads=[py, rows], writes=[r])
        fw.op("dve", lambda r=r, xt=xt: nc.vector.scalar_tensor_tensor(r[:], xt[:], ALPHA, r[:], ALU.mult, ALU.add),
              reads=[xt, r], writes=[r])
        if dbg == "A1":
            return
        x1 = x1_r.next()
        ln_tile(fw, nc, r, st_r.next(), rows, 1, 2, x1)
        fw.dma("sp", x1_d[T * 128:(T + 1) * 128, :], x1[:], reads=[x1], writes=[x1_d])
        if dbg == "A2":
            return
        h = h_r.next()
        mod_tile(fw, nc, x1, rows, 3, 4, h)
        hT32 = hT32_r.next()
        if dbg == "A3a":
            return
        if dbg == "A3b":
            transpose_tile(fw, nc, h, ident_f, pst.next(), [])
            return
        if dbg == "A3c":
            transpose_tile(fw, nc, h, ident_f, pst.next(), [("act", hT[:, :, T * 128:(T + 1) * 128], hT)])
            return
        if dbg == "A3d":
            transpose_tile(fw, nc, h, ident_f, pst.next(), [("dve", hT32[:], hT32)])
            return
        if dbg == "A3":
            transpose_tile(fw, nc, h, ident_f, pst.next(),
                           [("act", hT[:, :, T * 128:(T + 1) * 128], hT), ("dve", hT32[:], hT32)])
            return
        transpose_tile(fw, nc, h, ident_f, pst.next(),
                       [("act", hT[:, :, T * 128:(T + 1) * 128], hT), ("dve", hT32[:], hT32)])
        router_tile(fw, nc, hT32, wr_sb, brow, psl.next(), rt_r.next(), cw[:, T, :])
        cw_buf.w = rt_r.bufs[(rt_r.i - 1) % 2].w


def ffn_ln_phase(fw, nc, yacc, x1_d, rows, out_d):
    x_r = fw.ring(2, [128, D_MODEL], F32, "xc")
    r_r = fw.ring(2, [128, D_MODEL], F32, "rc")
    o_r = fw.ring(2, [128, D_MODEL], F32, "oc")
    st_r = fw.ring(2, [128, 16], F32, "stc")
    xt_next = x_r.next()
    fw.dma("sp", xt_next[:], x1_d[0:128, :], reads=[x1_d], writes=[xt_next])
    for T in range(NT):
        xt = xt_next
        if T + 1 < NT:
            xt_next = x_r.next()
            fw.dma("sp", xt_next[:], x1_d[(T + 1) * 128:(T + 2) * 128, :], reads=[x1_d], writes=[xt_next])
        r = r_r.next()
        fw.op("pool", lambda r=r, T=T: nc.gpsimd.tensor_mul(r[:], yacc[:, T, :], rows[:, 0, :]), reads=[yacc, rows], writes=[r])
        fw.op("dve", lambda r=r, xt=xt: nc.vector.scalar_tensor_tensor(r[:], xt[:], ALPHA, r[:], ALU.mult, ALU.add),
              reads=[xt, r], writes=[r])
        o = o_r.next()
        ln_tile(fw, nc, r, st_r.next(), rows, 1, 2, o)
        fw.dma("sp", out_d[T * 128:(T + 1) * 128, :], o[:], reads=[o], writes=[out_d])


TOK = 2048


def build_mid(stop=None):
    nc = new_nc()
    fw = FW(nc)
    hgT = fw.dram_in("hgT", [2048, TOK], BF16)
    x_d = fw.dram_in("x", [TOK, D_MODEL], F32)
    rows_d = fw.dram_in("rows", [12, D_MODEL], F32)
    w_out = fw.dram_in("w_out", [2048, D_MODEL], F32)
    wr_d = fw.dram_in("w_router", [D_MODEL, NE], F32)
    br_d = fw.dram_in("b_router", [1, NE], F32)
    wg_d = fw.dram_in("wg", [NE, D_MODEL, DFF], F32)
    wu_d = fw.dram_in("wu", [NE, D_MODEL, DFF], F32)
    wd_d = fw.dram_in("wd", [NE, DFF, D_MODEL], F32)
    wkv_d = fw.dram_in("w_kv", [D_MODEL, 2 * D_MODEL], F32)
    wq_d = fw.dram_in("wq1", [D_MODEL, D_MODEL], F32)
    cst_d = fw.dram_in("cst", [128, 128 + 8 * 16], F32)
    x2_o = fw.dram_out("x2", [TOK, D_MODEL], F32)
    KT_o = fw.dram_out("KT", [D_MODEL, TOK], BF16)
    V_o = fw.dram_out("V", [TOK, D_MODEL], BF16)
    km_o = fw.dram_out("kmT", [D_MODEL, 8], F32)
    QT_o = fw.dram_out("QT", [D_MODEL, TOK], BF16)
    kn_o = fw.dram_out("kn2", [16, 4], F32)
    x1_d = fw.dram_tmp("x1_tmp", [TOK, D_MODEL], F32)

    cst = fw.sbuf([128, 128 + 128], F32, "cst_sb")
    hsel_b = fw.sbuf([128, 8, 16], BF16, "hsel_b")
    wr_sb = fw.sbuf([128, 8, NE], F32, "wr_sb")
    brow = fw.sbuf([128, NE], F32, "brow")
    cw = fw.sbuf([128, NT, NE], F32, "cw")
    fw.dma("sp", cst[:], cst_d[:], writes=[cst])
    fw.dma("pool", hsel_b[:], cst_d[:, 128:256].rearrange("p (k h) -> p k h", k=8), writes=[hsel_b])
    fw.dma("sp", wr_sb[:], wr_d[:].rearrange("(k p) e -> p k e", p=128), writes=[wr_sb])
    fw.dma("sp", brow[:], br_d[0:1, :].to_broadcast([128, NE]), writes=[brow])

    m_h = fw.scope_begin()
    hT = fw.sbuf([128, 8, TOK], BF16, "hT")
    m_a = fw.scope_begin()
    wo_sb = fw.sbuf([128, 16, D_MODEL], BF16, "wo_sb")
    for k in range(16):
        fw.dma("pool", wo_sb[:, k, :], w_out[k * 128:(k + 1) * 128, :], writes=[wo_sb])
    rowsA = load_rows(fw, nc, rows_d, [0, 1, 2, 3, 4], "rowsA", plus1=(0, 3))
    hg_r = fw.ring(2, [128, 16, 128], BF16, "hg_t")
    hg_pending = {}

    def hg_load(T):
        hg = hg_r.next()
        fw.dma("sp", hg[:], hgT[:].rearrange("(k p) t -> p k t", p=128)[:, :, T * 128:(T + 1) * 128], writes=[hg])
        hg_pending[T] = hg

    hg_load(0)

    def y_fn(T, py):
        hg = hg_pending.pop(T)
        if T + 1 < NT:
            hg_load(T + 1)
        for half in range(2):
            fw.mm(py[:, half * 512:(half + 1) * 512],
                  [(hg[:, k, :], wo_sb[:, k, half * 512:(half + 1) * 512]) for k in range(16)], reads=[hg, wo_sb], writes=[py])

    mix_ln_phase(fw, nc, y_fn, x_d, x1_d, rowsA, hT, cw, cw, wr_sb, brow, cst,
                 dbg=(stop if stop in ("A0", "A1", "A2", "A3", "A4", "A3a", "A3b", "A3c", "A3d") else None))
    fw.scope_end(m_a)
    if stop is not None and stop.startswith("A"):
        fw.barrier([x1_d]); fw.close(); return nc
    yacc = fw.sbuf([128, NT, D_MODEL], F32, "yacc")
    moe_dense(fw, nc, hT, cw, cw, wg_d, wu_d, wd_d, yacc)
    if stop == "B":
        fw.barrier([x1_d]); fw.close(); return nc
    rowsC = load_rows(fw, nc, rows_d, [5, 6, 7], "rowsC", plus1=(5,))
    ffn_ln_phase(fw, nc, yacc, x1_d, rowsC, x2_o)
    fw.scope_end(m_h)
    if stop == "C":
        fw.barrier([x2_o]); fw.close(); return nc
    rowsD = load_rows(fw, nc, rows_d, [8, 9, 10, 11], "rowsD", plus1=(8, 10))
    wkv_sb = fw.sbuf([128, 8, 2 * D_MODEL], BF16, "wkv_sb")
    wq_sb = fw.sbuf([128, 8, D_MODEL], BF16, "wq_sb")
    for k in range(8):
        fw.dma("pool", wkv_sb[:, k, :], wkv_d[k * 128:(k + 1) * 128, :], writes=[wkv_sb])
        fw.dma("pool", wq_sb[:, k, :], wq_d[k * 128:(k + 1) * 128, :], writes=[wq_sb])
    x_r = fw.ring(2, [128, D_MODEL], F32, "xd")
    h_r = fw.ring(2, [128, D_MODEL], F32, "hd")
    hkT_r = fw.ring(2, [128, 8, 512], BF16, "hkT")
    hqT_r = fw.ring(2, [128, 8, 512], BF16, "hqT")
    kT_r = fw.ring(1, [128, 8, 512], BF16, "kTd")
    ksq_r = fw.ring(1, [128, 8, 512], BF16, "ksq")
    qT_r = fw.ring(1, [128, 8, 512], BF16, "qTd")
    v_r = fw.ring(2, [128, D_MODEL], BF16, "vd")
    km_sb = fw.sbuf([128, 8, 8], F32, "km_sb")
    kn_sb = fw.sbuf([16, 4], F32, "kn_sb")
    pst = fw.ring(1, [128, D_MODEL], F32, "pstd", psum=True)
    psm = fw.ring(4, [128, 512], F32, "psm", psum=True)
    for g in range(NT // 4):
        gs = slice(g * 512, (g + 1) * 512)
        hkT, hqT = hkT_r.next(), hqT_r.next()
        for tt in range(4):
            T = g * 4 + tt
            xt = x_r.next()
            fw.dma("sp", xt[:], x2_o[T * 128:(T + 1) * 128, :], reads=[x2_o], writes=[xt])
            for (isc, ish, dstT) in ((0, 1, hkT), (2, 3, hqT)):
                h = h_r.next()
                mod_tile(fw, nc, xt, rowsD, isc, ish, h)
                transpose_tile(fw, nc, h, cst, pst.next(), [("act", dstT[:, :, tt * 128:(tt + 1) * 128], dstT)])
        kTg, qTg, ksq = kT_r.next(), qT_r.next(), ksq_r.next()
        for fc in range(8):
            p = psm.next()
            fw.mm(p[:], [(wkv_sb[:, k, fc * 128:(fc + 1) * 128], hkT[:, k, :]) for k in range(8)], reads=[wkv_sb, hkT], writes=[p])
            fw.op("act", lambda p=p, fc=fc: nc.scalar.copy(kTg[:, fc, :], p[:]), reads=[p], writes=[kTg])
            fw.op("act", lambda p=p, fc=fc: nc.scalar.activation(ksq[:, fc, :], p[:], AF.Square), reads=[p], writes=[ksq])
            fw.op("dve", lambda p=p, fc=fc, g=g: nc.vector.tensor_reduce(
                km_sb[:, fc, 2 * g:2 * g + 2], p[:].rearrange("p (b t) -> p b t", b=2), AX.X, ALU.add), reads=[p], writes=[km_sb])
        fw.dma("sp", KT_o[:].rearrange("(c p) t -> p c t", p=128)[:, :, gs], kTg[:], reads=[kTg], writes=[KT_o])
        p = psm.next()
        fw.mm(p[0:16, :], [(hsel_b[:, fc, :], ksq[:, fc, :]) for fc in range(8)], reads=[hsel_b, ksq], writes=[p])
        fw.op("dve", lambda p=p, g=g: nc.vector.reduce_max(kn_sb[:, g:g + 1], p[0:16, :], AX.X), reads=[p], writes=[kn_sb])
        for fc in range(8):
            p = psm.next()
            fw.mm(p[:], [(wq_sb[:, k, fc * 128:(fc + 1) * 128], hqT[:, k, :]) for k in range(8)], reads=[wq_sb, hqT], writes=[p])
            fw.op("act", lambda p=p, fc=fc: nc.scalar.mul(qTg[:, fc, :], p[:], 0.125), reads=[p], writes=[qTg])
        fw.dma("sp", QT_o[:].rearrange("(c p) t -> p c t", p=128)[:, :, gs], qTg[:], reads=[qTg], writes=[QT_o])
        for tt in range(4):
            T = g * 4 + tt
            vt = v_r.next()
            for half in range(2):
                p = psm.next()
                fw.mm(p[:], [(hkT[:, k, tt * 128:(tt + 1) * 128], wkv_sb[:, k, D_MODEL + half * 512:D_MODEL + (half + 1) * 512])
                             for k in range(8)], reads=[wkv_sb, hkT], writes=[p])
                fw.op("dve", lambda p=p, half=half, vt=vt: nc.vector.tensor_copy(vt[:, half * 512:(half + 1) * 512], p[:]),
                      reads=[p], writes=[vt])
            fw.dma("act", V_o[T * 128:(T + 1) * 128, :], vt[:], reads=[vt], writes=[V_o])
    fw.op("dve", lambda: nc.vector.tensor_scalar(km_sb[:], km_sb[:], 1.0 / 256.0, None, ALU.mult), reads=[km_sb], writes=[km_sb])
    fw.dma("sp", km_o[:].rearrange("(c p) b -> p c b", p=128), km_sb[:], reads=[km_sb], writes=[km_o])
    fw.dma("sp", kn_o[:], kn_sb[:], reads=[kn_sb], writes=[kn_o])
    for e in ("sp", "act"):
        fw.wait_all(e, [x2_o, KT_o, V_o, km_o, QT_o, kn_o])
    fw.close()
    return nc


def mid_consts():
    cst = np.zeros((128, 256), np.float32)
    cst[:, :128] = np.eye(128, dtype=np.float32)
    hs = np.zeros((128, 8, 16), np.float32)
    for k in range(8):
        for p in range(128):
            hs[p, k, (k * 128 + p) // 64] = 1.0
    cst[:, 128:] = hs.reshape(128, 128)
    return cst


def cond_rows(inp, cond, b):
    g = lambda l, s, w: cond[b, mod_off(l, s, w):mod_off(l, s, w) + D_MODEL]
    kvo = 2 * 6 * D_MODEL
    return [g(0, 0, 2), inp["ln_g"][0, 0], inp["ln_b"][0, 0], g(0, 1, 1), g(0, 1, 0),
            g(0, 1, 2), inp["ln_g"][0, 1], inp["ln_b"][0, 1],
            cond[b, kvo + D_MODEL:kvo + 2 * D_MODEL], cond[b, kvo:kvo + D_MODEL],
            g(1, 0, 1), g(1, 0, 0)]


def run_mid(inp, cond, core_res):
    maps = []
    cst = mid_consts()
    for c in range(NCORES):
        b, seg = divmod(c, 4)
        ts = slice(seg * TOK, (seg + 1) * TOK)
        hgT = np.ascontiguousarray(np.concatenate([core_res[b * 4 + hd]["hgT"][:, ts] for hd in range(4)], axis=0))
        maps.append({"hgT": hgT, "x": np.ascontiguousarray(inp["x"][b, ts]),
                     "rows": np.ascontiguousarray(np.stack(cond_rows(inp, cond, b)).astype(np.float32)),
                     "w_out": inp["a_w_out"][0], "w_router": inp["moe_w_router"], "b_router": inp["moe_b_router"][None, :],
                     "wg": inp["moe_w_gate"][0], "wu": inp["moe_w_up"][0], "wd": inp["moe_w_down"][0],
                     "w_kv": inp["b_w_kv"], "wq1": inp["b_wq"][0], "cst": cst})
    return run(build_mid(), maps)


NBLK = 32
NAUX = 33


def build_select():
    nc = new_nc()
    fw = FW(nc)
    QT = fw.dram_in("QT", [D_MODEL, TOK], BF16)
    km_d = fw.dram_in("kmT", [D_MODEL, NBLK], F32)
    kn_d = fw.dram_in("kn2", [1, 16 * 16], F32)
    tab_d = fw.dram_in("tab", [128, NT, 2 * NBLK + 16], F32)
    cst_d = fw.dram_in("cst", [128, 256], F32)
    aux_o = fw.dram_out("auxR", [16, NAUX, TOK], BF16)
    QT_sb = fw.sbuf([128, 8, TOK], BF16, "QT_sb")
    Qsq = fw.sbuf([128, 8, TOK], BF16, "Qsq")
    km_b = fw.sbuf([128, 8, NBLK], BF16, "km_b")
    tab = fw.sbuf([128, NT, 2 * NBLK + 16], F32, "tab")
    cst = fw.sbuf([128, 256], F32, "cst_sb")
    hsel_b = fw.sbuf([128, 8, 16], BF16, "hsel_b")
    knb = fw.sbuf([128, 16, 16], F32, "knb")
    knmax = fw.sbuf([128, 16], F32, "knmax")
    fw.dma("sp", QT_sb[:], QT[:].rearrange("(c p) t -> p c t", p=128), writes=[QT_sb])
    fw.dma("pool", km_b[:], km_d[:].rearrange("(c p) n -> p c n", p=128), writes=[km_b])
    fw.dma("sp", tab[:], tab_d[:], writes=[tab])
    fw.dma("sp", cst[:], cst_d[:], writes=[cst])
    fw.dma("pool", hsel_b[:], cst_d[:, 128:256].rearrange("p (k h) -> p k h", k=8), writes=[hsel_b])
    fw.dma("sp", knb[:].rearrange("p h v -> p (h v)"), kn_d[0:1, :].to_broadcast([128, 256]), writes=[knb])
    fw.op("dve", lambda: nc.vector.tensor_reduce(knmax[:], knb[:], AX.X, ALU.max), reads=[knb], writes=[knmax])
    for c in range(8):
        fw.op("pool", lambda c=c: nc.gpsimd.tensor_mul(Qsq[:, c, :], QT_sb[:, c, :], QT_sb[:, c, :]), reads=[QT_sb], writes=[Qsq])
    pg_r = fw.ring(2, [128, 512], F32, "pg", psum=True)
    pn_r = fw.ring(1, [128, 512], F32, "pn", psum=True)
    ptr_r = fw.ring(4, [128, 512], F32, "ptr", psum=True)
    gm_r = fw.ring(2, [128, 16, NBLK], F32, "gm")
    t8_r = fw.ring(2, [128, 16, 8], F32, "t8")
    mt_r = fw.ring(2, [128, 16, NAUX], F32, "mt")
    mm_r = fw.ring(2, [128, 16], F32, "mq")
    ax_r = fw.ring(2, [NAUX, 16, 128], BF16, "ax")
    V = nc.vector
    for T in range(NT):
        qs = slice(T * 128, (T + 1) * 128)
        pg = pg_r.next()
        for h in range(16):
            pb = (h % 2) * 64
            fw.mm(pg[:, h * NBLK:(h + 1) * NBLK], [(QT_sb[pb:pb + 64, h // 2, qs], km_b[pb:pb + 64, h // 2, :])],
                  reads=[QT_sb, km_b], writes=[pg])
        gm, t8, mt, mq = gm_r.next(), t8_r.next(), mt_r.next(), mm_r.next()
        pastb = tab[:, T, 0:NBLK].unsqueeze(1).to_broadcast([128, 16, NBLK])
        notown = tab[:, T, NBLK:2 * NBLK].unsqueeze(1).to_broadcast([128, 16, NBLK])
        fw.op("dve", lambda: V.tensor_tensor(gm[:], pg[:].rearrange("p (h n) -> p h n", h=16), pastb, ALU.add),
              reads=[pg, tab], writes=[gm])
        for h in range(16):
            fw.op("dve", lambda h=h: V.max(t8[:, h, :], gm[:, h, :]), reads=[gm], writes=[t8])
        fw.op("dve", lambda: V.tensor_tensor(gm[:], gm[:], t8[:, :, 2:3].to_broadcast([128, 16, NBLK]), ALU.is_ge),
              reads=[gm, t8], writes=[gm])
        fw.op("dve", lambda: V.tensor_scalar(gm[:], gm[:], -1.0, BIG, ALU.add, ALU.mult), reads=[gm], writes=[gm])
        fw.op("dve", lambda: V.tensor_tensor(gm[:], gm[:], pastb, ALU.min), reads=[gm, tab], writes=[gm])
        fw.op("dve", lambda: V.tensor_tensor(mt[:, :, 0:NBLK], gm[:], notown, ALU.mult), reads=[gm, tab], writes=[mt])
        pn = pn_r.next()
        fw.mm(pn[:, 0:16], [(Qsq[:, c, qs], hsel_b[:, c, :]) for c in range(8)], reads=[Qsq, hsel_b], writes=[pn])
        fw.op("dve", lambda: V.tensor_tensor(mq[:], pn[:, 0:16], knmax[:], ALU.mult), reads=[pn, knmax], writes=[mq])
        fw.op("act", lambda: nc.scalar.activation(mq[:], mq[:], AF.Sqrt), reads=[mq], writes=[mq])
        fw.op("dve", lambda: V.scalar_tensor_tensor(mt[:, :, NBLK], mq[:], -1.0, tab[:, T, 2 * NBLK:2 * NBLK + 16],
                                                    ALU.mult, ALU.subtract), reads=[mq, tab], writes=[mt])
        ax = ax_r.next()
        for hq in range(4):
            pt = ptr_r.next()
            for i in range(4):
                h = hq * 4 + i
                fw.op("pe", lambda h=h, i=i, pt=pt: nc.tensor.transpose(pt[0:NAUX, i * 128:(i + 1) * 128], mt[:, h, :], cst[:, 0:128]),
                      reads=[mt, cst], writes=[pt], inc=(i == 3))
            fw.op("act", lambda pt=pt, hq=hq: nc.scalar.copy(ax[:, hq * 4:(hq + 1) * 4, :],
                                                             pt[0:NAUX, :].rearrange("p (h t) -> p h t", h=4)),
                  reads=[pt], writes=[ax])
        fw.dma("sp", aux_o[:].rearrange("h r t -> r h t")[:, :, qs], ax[:], reads=[ax], writes=[aux_o])
    fw.wait_all("sp", [aux_o])
    fw.close()
    return nc


def alibi_slopes():
    return np.exp2(-8.0 * (np.arange(16, dtype=np.float32) + 1.0) / 16).astype(np.float32)


def run_select(mid):
    maps = []
    cst = mid_consts()
    sl = alibi_slopes()
    for c in range(NCORES):
        b, seg = divmod(c, 4)
        kmT = np.ascontiguousarray(np.concatenate([mid[b * 4 + s]["kmT"] for s in range(4)], axis=1))
        kn2 = np.ascontiguousarray(np.concatenate([mid[b * 4 + s]["kn2"] for s in range(4)], axis=1).reshape(1, 256))
        tab = np.zeros((128, NT, 2 * NBLK + 16), np.float32)
        for T in range(NT):
            own = (seg * TOK + T * 128) // 256
            tab[:, T, 0:NBLK] = np.where(np.arange(NBLK) < own, 0.0, -BIG)[None, :]
            tab[:, T, NBLK:2 * NBLK] = np.where(np.arange(NBLK) == own, 0.0, 1.0)[None, :]
            iq = (T % 4) * 128 + np.arange(128)
            tab[:, T, 2 * NBLK:] = sl[None, :] * iq[:, None]
        maps.append({"QT": mid[c]["QT"], "kmT": kmT, "kn2": kn2, "tab": tab, "cst": cst})
    return run(build_select(), maps)


NQG = SEQ // 512
NKA = 16


def build_attn(nheads=16):
    nc = new_nc()
    fw = FW(nc)
    KTr = fw.dram_in("KTr", [D_MODEL, NKA * 128], BF16)
    Vr = fw.dram_in("Vr", [16, 128, NKA * 64], BF16)
    QT = fw.dram_in("QT", [D_MODEL, SEQ], BF16)
    aux = fw.dram_in("auxR", [16, NAUX, SEQ], BF16)
    auxK = fw.dram_in("auxK", [NAUX, NKA * 128], BF16)
    bias_d = fw.dram_in("biasT", [128, 16 * NKA], F32)
    cst_d = fw.dram_in("cst", [128, 128 + 512], F32)
    Op = fw.dram_out("Opart", [16, 65, SEQ], F32)
    bias = fw.sbuf([128, 16 * NKA], F32, "bias")
    cb = fw.sbuf([128, 128 + 512], BF16, "cb")
    fw.dma("sp", bias[:], bias_d[:], writes=[bias])
    fw.dma("pool", cb[:], cst_d[:], writes=[cb])
    KL_r = fw.ring(2, [128, NKA * 128], BF16, "KL")
    Vx_r = fw.ring(2, [128, NKA, 65], BF16, "Vx")
    QR_r = fw.ring(3, [128, 512], BF16, "QR")
    PT_r = fw.ring(4, [128, 512], BF16, "PT")
    os_r = fw.ring(2, [128, 512], F32, "osb")
    psS = fw.ring(3, [128, 512], F32, "psS", psum=True)
    psO = fw.ring(2, [128, 512], F32, "psO", psum=True)
    for KL in KL_r.bufs:
        fw.dma("sp", KL[64:64 + NAUX, :], auxK[:], writes=[KL])
    for Vx in Vx_r.bufs:
        fw.op("pool", lambda Vx=Vx: nc.gpsimd.memset(Vx[:], 1.0), writes=[Vx])
    def load_head(h):
        KL, Vx = KL_r.next(), Vx_r.next()
        fw.dma("sp", KL[0:64, :], KTr[h * 64:(h + 1) * 64, :], writes=[KL])
        fw.dma("sp", Vx[:, :, 0:64], Vr[h].rearrange("p (a d) -> p a d", a=NKA), writes=[Vx])
        return KL, Vx

    def load_q(h, j):
        qs = slice(j * 512, (j + 1) * 512)
        QR = QR_r.next()
        fw.dma("sp", QR[0:64, :], QT[h * 64:(h + 1) * 64, qs], writes=[QR])
        fw.dma("sp", QR[64:64 + NAUX, :], aux[h, :, qs], writes=[QR])
        return QR

    steps = [(h, j) for h in range(nheads) for j in range(NQG)]
    head_next = load_head(0)
    q_next = load_q(0, 0)
    for si, (h, j) in enumerate(steps):
        if j == 0:
            KL, Vx = head_next
            if h + 1 < nheads:
                head_next = load_head(h + 1)
        QR = q_next
        if si + 1 < len(steps):
            q_next = load_q(*steps[si + 1])
        qs = slice(j * 512, (j + 1) * 512)
        po = psO.next()

        def qk(a):
            ps = psS.next()
            pairs = [(KL[0:64 + NAUX, a * 128:(a + 1) * 128], QR[0:64 + NAUX, :])]
            if a == j:
                pairs.append((cb[:, 0:128], cb[:, 128:640]))
            fw.mm(ps[:], pairs, reads=[KL, QR, cb], writes=[ps])
            return ps

        ps_cur = qk(0)
        for a in range(j + 1):
            ps = ps_cur
            if a < j:
                ps_cur = qk(a + 1)
            PT = PT_r.next()
            fw.op("act", lambda ps=ps, PT=PT, h=h, j=j, a=a: nc.scalar.activation(
                PT[:], ps[:], AF.Exp, bias=bias[:, h * NKA + (j - a):h * NKA + (j - a) + 1]),
                reads=[ps, bias], writes=[PT])
            fw.op("pe", lambda po=po, Vx=Vx, PT=PT, a=a, j=j: nc.tensor.matmul(
                po[0:65, :], Vx[:, a, :], PT[:], start=(a == 0), stop=(a == j)),
                reads=[Vx, PT], writes=[po], inc=True)
        osb = os_r.next()
        fw.op("dve", lambda osb=osb, po=po: nc.vector.tensor_copy(osb[0:65, :], po[0:65, :]), reads=[po], writes=[osb])
        fw.dma("sp", Op[h, :, qs], osb[0:65, :], reads=[osb], writes=[Op])
    fw.wait_all("sp", [Op])
    fw.close()
    return nc


def run_attn(mid, sel, nheads=16):
    maps = []
    sl = alibi_slopes()
    for c in range(NCORES):
        b, r = divmod(c, 4)
        KT = np.concatenate([mid[b * 4 + s]["KT"] for s in range(4)], axis=1)
        V = np.concatenate([mid[b * 4 + s]["V"] for s in range(4)], axis=0)
        QT = np.ascontiguousarray(np.concatenate([mid[b * 4 + s]["QT"] for s in range(4)], axis=1))
        aux = np.ascontiguousarray(np.concatenate([sel[b * 4 + s]["auxR"] for s in range(4)], axis=2))
        kts = [4 * a + r for a in range(NKA)]
        KTr = np.ascontiguousarray(np.concatenate([KT[:, kt * 128:(kt + 1) * 128] for kt in kts], axis=1))
        Vsel = np.stack([V[kt * 128:(kt + 1) * 128, :] for kt in kts], axis=0)
        Vr = np.ascontiguousarray(Vsel.reshape(NKA, 128, 16, 64).transpose(2, 1, 0, 3).reshape(16, 128, NKA * 64))
        auxK = np.zeros((NAUX, NKA * 128), np.float32)
        for a, kt in enumerate(kts):
            auxK[kt // 2, a * 128:(a + 1) * 128] = 1.0
        auxK[NBLK, :] = 1.0
        ik = np.arange(128, dtype=np.float32)
        biasT = np.zeros((128, 16, NKA), np.float32)
        for dj in range(NKA):
            biasT[:, :, dj] = sl[None, :] * (ik[:, None] + 128.0 * r - 512.0 * dj)
        cst = np.zeros((128, 128 + 512), np.float32)
        cst[:, :128] = np.eye(128)
        kk, qq = np.meshgrid(np.arange(128) + 128 * r, np.arange(512), indexing="ij")
        same_blk = (kk // 256) == (qq // 256)
        cst[:, 128:] = np.where(same_blk & (kk > qq), -BIG, 0.0)
        maps.append({"KTr": KTr, "Vr": Vr, "QT": QT, "auxR": aux, "auxK": auxK.astype(NPBF),
                     "biasT": np.ascontiguousarray(biasT.reshape(128, 16 * NKA)), "cst": cst})
    return run(build_attn(nheads), maps)


def build_tail():
    nc = new_nc()
    fw = FW(nc)
    Op = fw.dram_in("Op4", [4, 16, 65, TOK], F32)
    x_d = fw.dram_in("x2", [TOK, D_MODEL], F32)
    rows_d = fw.dram_in("rows", [8, D_MODEL], F32)
    wo_d = fw.dram_in("wo", [D_MODEL, D_MODEL], F32)
    wr_d = fw.dram_in("w_router", [D_MODEL, NE], F32)
    br_d = fw.dram_in("b_router", [1, NE], F32)
    wg_d = fw.dram_in("wg", [NE, D_MODEL, DFF], F32)
    wu_d = fw.dram_in("wu", [NE, D_MODEL, DFF], F32)
    wd_d = fw.dram_in("wd", [NE, DFF, D_MODEL], F32)
    cst_d = fw.dram_in("cst", [128, 256], F32)
    out_o = fw.dram_out("out", [TOK, D_MODEL], F32)
    x3_d = fw.dram_tmp("x3_tmp", [TOK, D_MODEL], F32)

    cst = fw.sbuf([128, 256], F32, "cst_sb")
    wr_sb = fw.sbuf([128, 8, NE], F32, "wr_sb")
    brow = fw.sbuf([128, NE], F32, "brow")
    cw = fw.sbuf([128, NT, NE], F32, "cw")
    fw.dma("sp", cst[:], cst_d[:], writes=[cst])
    fw.dma("sp", wr_sb[:], wr_d[:].rearrange("(k p) e -> p k e", p=128), writes=[wr_sb])
    fw.dma("sp", brow[:], br_d[0:1, :].to_broadcast([128, NE]), writes=[brow])
    O = fw.sbuf([128, NT, D_MODEL], F32, "O_tm")
    m_h = fw.scope_begin()
    hT = fw.sbuf([128, 8, TOK], BF16, "hT")
    m_a = fw.scope_begin()
    m_o = fw.scope_begin()
    op_r = fw.ring(2, [65, 4, TOK // 2], F32, "op4")
    os_r = fw.ring(2, [65, TOK // 2], F32, "osum")
    rl_r = fw.ring(2, [128, 4], F32, "rl")
    ptr = fw.ring(2, [128, 4, 128], F32, "ptr6", psum=True)
    for h in range(16):
        for hf in range(2):
            cs = slice(hf * (TOK // 2), (hf + 1) * (TOK // 2))
            o4, osum = op_r.next(), os_r.next()
            for r in range(4):
                fw.dma("sp" if r % 2 == 0 else "act", o4[:, r, :], Op[r, h, :, cs], writes=[o4])
            fw.op("dve", lambda o4=o4, osum=osum: nc.vector.tensor_add(osum[:], o4[:, 0, :], o4[:, 1, :]), reads=[o4], writes=[osum])
            fw.op("pool", lambda o4=o4: nc.gpsimd.tensor_add(o4[:, 2, :], o4[:, 2, :], o4[:, 3, :]), reads=[o4], writes=[o4])
            fw.op("dve", lambda o4=o4, osum=osum: nc.vector.tensor_add(osum[:], osum[:], o4[:, 2, :]), reads=[o4, osum], writes=[osum])
            for tq in range(2):
                pt, rl = ptr.next(), rl_r.next()
                for i in range(4):
                    ti = tq * 4 + i
                    fw.op("pe", lambda pt=pt, i=i, ti=ti, osum=osum: nc.tensor.transpose(
                        pt[:, i, 0:65], osum[:, ti * 128:(ti + 1) * 128], cst[0:65, 0:65]),
                        reads=[osum, cst], writes=[pt], inc=(i == 3))
                fw.op("dve", lambda pt=pt, rl=rl: nc.vector.reciprocal(rl[:], pt[:, :, 64]), reads=[pt], writes=[rl])
                T0 = hf * 8 + tq * 4
                fw.op("dve", lambda pt=pt, rl=rl, T0=T0, h=h: nc.vector.tensor_tensor(
                    O[:, T0:T0 + 4, h * 64:(h + 1) * 64], pt[:, :, 0:64], rl[:].unsqueeze(2).to_broadcast([128, 4, 64]), ALU.mult),
                    reads=[pt, rl], writes=[O])
    fw.scope_end(m_o)
    wo_sb = fw.sbuf([128, 8, D_MODEL], BF16, "wo_sb")
    for k in range(8):
        fw.dma("pool", wo_sb[:, k, :], wo_d[k * 128:(k + 1) * 128, :], writes=[wo_sb])
    rowsA = load_rows(fw, nc, rows_d, [0, 1, 2, 3, 4], "rowsA", plus1=(0, 3))
    OT_r = fw.ring(2, [128, 8, 128], BF16, "OT")
    pst = fw.ring(1, [128, D_MODEL], F32, "pst", psum=True)

    def y_fn(T, py):
        OT = OT_r.next()
        for k in range(8):
            fw.op("pe", lambda k=k: nc.tensor.transpose(pst.bufs[0][:, k * 128:(k + 1) * 128], O[:, T, k * 128:(k + 1) * 128],
                                                        cst[:, 0:128]), reads=[O, cst], writes=[pst.bufs[0]], inc=(k == 7))
        fw.op("act", lambda: nc.scalar.copy(OT[:], pst.bufs[0][:].rearrange("p (k t) -> p k t", k=8)),
              reads=[pst.bufs[0]], writes=[OT])
        for half in range(2):
            fw.mm(py[:, half * 512:(half + 1) * 512],
                  [(OT[:, k, :], wo_sb[:, k, half * 512:(half + 1) * 512]) for k in range(8)], reads=[OT, wo_sb], writes=[py])

    mix_ln_phase(fw, nc, y_fn, x_d, x3_d, rowsA, hT, cw, cw, wr_sb, brow, cst, pst=pst)
    fw.scope_end(m_a)
    yacc = O
    moe_dense(fw, nc, hT, cw, cw, wg_d, wu_d, wd_d, yacc)
    rowsC = load_rows(fw, nc, rows_d, [5, 6, 7], "rowsC", plus1=(5,))
    ffn_ln_phase(fw, nc, yacc, x3_d, rowsC, out_o)
    fw.scope_end(m_h)
    fw.wait_all("sp", [out_o])
    fw.close()
    return nc


def run_tail(inp, cond, mid, att):
    maps = []
    cst = mid_consts()
    for c in range(NCORES):
        b, seg = divmod(c, 4)
        ts = slice(seg * TOK, (seg + 1) * TOK)
        Op4 = np.ascontiguousarray(np.stack([att[b * 4 + r]["Opart"][:, :, ts] for r in range(4)]))
        g = lambda l, s, w: cond[b, mod_off(l, s, w):mod_off(l, s, w) + D_MODEL]
        rows = np.stack([g(1, 0, 2), inp["ln_g"][1, 0], inp["ln_b"][1, 0], g(1, 1, 1), g(1, 1, 0),
                         g(1, 1, 2), inp["ln_g"][1, 1], inp["ln_b"][1, 1]]).astype(np.float32)
        maps.append({"Op4": Op4, "x2": mid[c]["x2"], "rows": np.ascontiguousarray(rows), "wo": inp["b_wo"][0],
                     "w_router": inp["moe_w_router"], "b_router": inp["moe_b_router"][None, :],
                     "wg": inp["moe_w_gate"][1], "wu": inp["moe_w_up"][1], "wd": inp["moe_w_down"][1], "cst": cst})
    return run(build_tail(), maps)


def kernel(**inp):
    inp = {k: np.asarray(v) for k, v in inp.items()}
    cond = run_cond(inp)
    front = run_mlstm_front(inp, cond)
    core = run_mlstm_core(inp, front)
    del front
    mid = run_mid(inp, cond, core)
    del core
    sel = run_select(mid)
    att = run_attn(mid, sel)
    res = run_tail(inp, cond, mid, att)
    out = np.empty((2, SEQ, D_MODEL), np.float32)
    for c in range(NCORES):
        b, seg = divmod(c, 4)
        out[b, seg * TOK:(seg + 1) * TOK] = res[c]["out"]
    return out
```
